# Optimizing a Trainium2 kernel written in Bass

```python
import numpy as np
import jax
import jax.numpy as jnp
from jax import lax

D_MODEL = 1024
BATCH = 4
SEQ = 4096
DEPTH = 2

HEAD_DIM = 64
N_HEADS = D_MODEL // HEAD_DIM
A_HEADS = N_HEADS // 2
B_Q_HEADS = N_HEADS // 2
B_KV_HEADS = max(1, B_Q_HEADS // 4)
C_HEADS = N_HEADS
DIL_CONFIGS = ((128, 1), (512, 4), (2048, 16))
A_BLOCK = 128
B_WINDOW = 128
B_BLOCK = 128
GRID_W = 64
NA_ROWS = 8
NA_COLS = 16
NA_COL_BLOCK = 16
NA_COL_SPAN = 32
D_FF_DENSE = 128 * ((8 * D_MODEL // 3 + 127) // 128)
N_EXPERTS = 8
TOP_K = 2
D_FF_EXPERT = 7 * D_MODEL // 2
RMS_EPS = 1e-6
NEG_INF = -1e30
EVEN_SPLITS = (A_HEADS * HEAD_DIM, A_HEADS * HEAD_DIM, A_HEADS * HEAD_DIM,
               B_Q_HEADS * HEAD_DIM, B_KV_HEADS * HEAD_DIM, B_KV_HEADS * HEAD_DIM)
EVEN_IN = sum(EVEN_SPLITS)
EVEN_OUT = (A_HEADS + B_Q_HEADS) * HEAD_DIM
N_EVEN = (DEPTH + 1) // 2
N_ODD = DEPTH // 2

kernel_name = 'hybrid_dilated_swa_natten_moe_encoder'


def rms_norm(x, g):
    xf = x.astype(jnp.float32)
    y = xf * lax.rsqrt(jnp.mean(xf * xf, axis=-1, keepdims=True) + RMS_EPS)
    return (y * g.astype(jnp.float32)).astype(x.dtype)


def alibi_slopes():
    s = np.exp2(-8.0 * np.arange(1, N_HEADS + 1) / N_HEADS).astype(np.float32)
    return jnp.asarray(s[0::2][:A_HEADS]), jnp.asarray(s[1::2][:B_Q_HEADS])


def to_heads(t, n):
    b, s, _ = t.shape
    return t.reshape(b, s, n, HEAD_DIM).transpose(0, 2, 1, 3)


def banded_attention(q, k, v, radius, block, pos_scale, slopes, sink=None):
    n, hq, seq_len, hd = q.shape
    hkv = k.shape[1]
    g = hq // hkv
    blk = min(block, seq_len)
    nb = -(-seq_len // blk)
    lp = nb * blk
    span = blk + 2 * radius
    qb = jnp.pad(q, ((0, 0), (0, 0), (0, lp - seq_len), (0, 0))).reshape(n, hkv, g, nb, blk, hd)
    pad = ((0, 0), (0, 0), (radius, lp - seq_len + radius), (0, 0))
    idx = np.arange(nb)[:, None] * blk + np.arange(span)[None, :]
    kb = jnp.take(jnp.pad(k, pad), idx.reshape(-1), axis=2).reshape(n, hkv, nb, span, hd)
    vb = jnp.take(jnp.pad(v, pad), idx.reshape(-1), axis=2).reshape(n, hkv, nb, span, hd)
    qpos = np.arange(nb)[:, None] * blk + np.arange(blk)[None, :]
    kpos = idx - radius
    dist = np.abs(qpos[:, :, None] - kpos[:, None, :])
    valid = (dist <= radius) & (kpos[:, None, :] >= 0) & (kpos[:, None, :] < seq_len)
    bias = -slopes.astype(jnp.float32).reshape(hkv, g, 1, 1, 1) * jnp.asarray(dist * pos_scale, jnp.float32)
    s = jnp.einsum('nkgbqd,nkbsd->nkgbqs', qb, kb).astype(jnp.float32) * (hd ** -0.5) + bias
    s = jnp.where(jnp.asarray(valid), s, NEG_INF)
    m = jnp.max(s, axis=-1)
    if sink is not None:
        sk = sink.astype(jnp.float32).reshape(hkv, g, 1, 1)
        m = jnp.maximum(m, sk)
    p = jnp.exp(s - m[..., None])
    l = jnp.sum(p, axis=-1)
    if sink is not None:
        l = l + jnp.exp(sk - m)
    o = jnp.einsum('nkgbqs,nkbsd->nkgbqd', p, vb.astype(jnp.float32)) / l[..., None]
    o = o.reshape(n, hq, lp, hd)[:, :, :seq_len]
    lse = (m + jnp.log(l)).reshape(n, hq, lp)[:, :, :seq_len]
    return o, lse


def dilated_attention(q, k, v, slopes):
    b, h, s, hd = q.shape
    outs, lses = [], []
    for window, r in DIL_CONFIGS:
        seg = s // r

        def fold(t):
            return t.reshape(b, h, seg, r, hd).transpose(0, 3, 1, 2, 4).reshape(b * r, h, seg, hd)

        o, lse = banded_attention(fold(q), fold(k), fold(v), radius=window // (2 * r),
                                  block=A_BLOCK, pos_scale=r, slopes=slopes)
        outs.append(o.reshape(b, r, h, seg, hd).transpose(0, 2, 3, 1, 4).reshape(b, h, s, hd))
        lses.append(lse.reshape(b, r, h, seg).transpose(0, 2, 3, 1).reshape(b, h, s))
    w = jax.nn.softmax(jnp.stack(lses), axis=0)
    return sum(w[i][..., None] * outs[i] for i in range(len(outs)))


def even_mixer(h, w_in, qn_a, kn_a, qn_b, kn_b, sink_b, w_out):
    b, s, _ = h.shape
    proj = h @ w_in
    qa, ka, va, qb, kb, vb = jnp.split(proj, np.cumsum(EVEN_SPLITS)[:-1].tolist(), axis=-1)
    qa = rms_norm(to_heads(qa, A_HEADS), qn_a)
    ka = rms_norm(to_heads(ka, A_HEADS), kn_a)
    va = to_heads(va, A_HEADS)
    qb = rms_norm(to_heads(qb, B_Q_HEADS), qn_b)
    kb = rms_norm(to_heads(kb, B_KV_HEADS), kn_b)
    vb = to_heads(vb, B_KV_HEADS)
    slopes_a, slopes_b = alibi_slopes()
    oa = dilated_attention(qa, ka, va, slopes_a)
    ob, _ = banded_attention(qb, kb, vb, radius=B_WINDOW, block=B_BLOCK, pos_scale=1,
                             slopes=slopes_b, sink=sink_b)
    o = jnp.concatenate([oa, ob], axis=1).transpose(0, 2, 1, 3).reshape(b, s, EVEN_OUT)
    return o.astype(h.dtype) @ w_out


def neighbourhood_mixer(h, w_qkv, qn, kn, rpb, w_out):
    b, s, _ = h.shape
    rows = s // GRID_W
    kh = min(NA_ROWS, rows)
    q, k, v = jnp.split(h @ w_qkv, 3, axis=-1)
    q = rms_norm(to_heads(q, C_HEADS), qn)
    k = rms_norm(to_heads(k, C_HEADS), kn)
    v = to_heads(v, C_HEADS)
    qg = q.reshape(b, C_HEADS, rows, GRID_W, HEAD_DIM)
    kg = k.reshape(b, C_HEADS, rows, GRID_W, HEAD_DIM)
    vg = v.reshape(b, C_HEADS, rows, GRID_W, HEAD_DIM)
    n_cb = GRID_W // NA_COL_BLOCK
    qcol = np.arange(GRID_W).reshape(n_cb, NA_COL_BLOCK)
    kcol = np.clip(qcol[:, 0] - NA_COLS // 2, 0, GRID_W - NA_COL_SPAN)[:, None] + np.arange(NA_COL_SPAN)
    qstart = np.clip(qcol - NA_COLS // 2, 0, GRID_W - NA_COLS)
    col_valid = jnp.asarray((kcol[:, None, :] >= qstart[:, :, None]) &
                            (kcol[:, None, :] < qstart[:, :, None] + NA_COLS))
    col_off = np.clip(kcol[:, None, :] - qcol[:, :, None], -(NA_COLS - 1), NA_COLS - 1) + NA_COLS - 1
    rpb_cols = jnp.take(rpb.astype(jnp.float32), col_off.reshape(-1), axis=2).reshape(
        C_HEADS, 2 * NA_ROWS - 1, n_cb, NA_COL_BLOCK, NA_COL_SPAN)
    kcol_flat = kcol.reshape(-1)

    def row_step(i):
        rs = jnp.clip(i - kh // 2, 0, rows - kh)
        qi = lax.dynamic_index_in_dim(qg, i, axis=2, keepdims=False).reshape(
            b, C_HEADS, n_cb, NA_COL_BLOCK, HEAD_DIM)
        kr = lax.dynamic_slice_in_dim(kg, rs, kh, axis=2)
        vr = lax.dynamic_slice_in_dim(vg, rs, kh, axis=2)
        kc = jnp.take(kr, kcol_flat, axis=3).reshape(b, C_HEADS, kh, n_cb, NA_COL_SPAN, HEAD_DIM)
        vc = jnp.take(vr, kcol_flat, axis=3).reshape(b, C_HEADS, kh, n_cb, NA_COL_SPAN, HEAD_DIM)
        sc = jnp.einsum('bhcqd,bhrckd->bhcqrk', qi, kc).astype(jnp.float32) * (HEAD_DIM ** -0.5)
        row_off = rs + jnp.arange(kh) - i + NA_ROWS - 1
        bias = jnp.take(rpb_cols, row_off, axis=1).transpose(0, 2, 3, 1, 4)
        sc = jnp.where(col_valid[:, :, None, :], sc + bias, NEG_INF)
        p = jax.nn.softmax(sc.reshape(sc.shape[:4] + (kh * NA_COL_SPAN,)), axis=-1).reshape(sc.shape)
        return jnp.einsum('bhcqrk,bhrckd->bhcqd', p, vc.astype(jnp.float32))

    o = lax.map(row_step, jnp.arange(rows))
    o = o.transpose(1, 2, 0, 3, 4, 5).reshape(b, C_HEADS, s, HEAD_DIM)
    o = o.transpose(0, 2, 1, 3).reshape(b, s, C_HEADS * HEAD_DIM)
    return o.astype(h.dtype) @ w_out


def swiglu(h, w_gate, w_up, w_down):
    return (jax.nn.silu(h @ w_gate) * (h @ w_up)) @ w_down


def moe_swiglu(h, w_router, w_gate, w_up, w_down):
    b, s, d = h.shape
    t = h.reshape(b * s, d)
    logits = (t @ w_router).astype(jnp.float32)
    top_v, top_i = lax.top_k(logits, TOP_K)
    gates = jax.nn.softmax(top_v, axis=-1)
    combine = jnp.sum(jax.nn.one_hot(top_i, N_EXPERTS, dtype=jnp.float32) * gates[..., None], axis=1)
    out = jnp.zeros((b * s, d), jnp.float32)
    for e in range(N_EXPERTS):
        y = swiglu(t, w_gate[e], w_up[e], w_down[e]).astype(jnp.float32)
        out = out + combine[:, e:e + 1] * y
    return out.reshape(b, s, d).astype(h.dtype)


def setup_inputs(seed: int = 0) -> dict:
    key = jax.random.key(seed)
    ks = iter(jax.random.split(key, 32))

    def nrm(shape, scale):
        return scale * jax.random.normal(next(ks), shape, jnp.float32)

    def gain(shape):
        return 1.0 + 0.05 * jax.random.normal(next(ks), shape, jnp.float32)

    d = D_MODEL
    return {
        'x': nrm((BATCH, SEQ, d), 1.0),
        'ev_norm1': gain((N_EVEN, d)),
        'ev_w_in': nrm((N_EVEN, d, EVEN_IN), d ** -0.5),
        'ev_qn_a': gain((N_EVEN, HEAD_DIM)),
        'ev_kn_a': gain((N_EVEN, HEAD_DIM)),
        'ev_qn_b': gain((N_EVEN, HEAD_DIM)),
        'ev_kn_b': gain((N_EVEN, HEAD_DIM)),
        'ev_sink_b': nrm((N_EVEN, B_Q_HEADS), 1.0),
        'ev_w_out': nrm((N_EVEN, EVEN_OUT, d), EVEN_OUT ** -0.5),
        'ev_norm2': gain((N_EVEN, d)),
        'ev_ffn_gate': nrm((N_EVEN, d, D_FF_DENSE), d ** -0.5),
        'ev_ffn_up': nrm((N_EVEN, d, D_FF_DENSE), d ** -0.5),
        'ev_ffn_down': nrm((N_EVEN, D_FF_DENSE, d), D_FF_DENSE ** -0.5),
        'od_norm1': gain((N_ODD, d)),
        'od_w_qkv': nrm((N_ODD, d, 3 * C_HEADS * HEAD_DIM), d ** -0.5),
        'od_qn': gain((N_ODD, HEAD_DIM)),
        'od_kn': gain((N_ODD, HEAD_DIM)),
        'od_rpb': nrm((N_ODD, C_HEADS, 2 * NA_ROWS - 1, 2 * NA_COLS - 1), 0.5),
        'od_w_out': nrm((N_ODD, C_HEADS * HEAD_DIM, d), (C_HEADS * HEAD_DIM) ** -0.5),
        'od_norm2': gain((N_ODD, d)),
        'od_router': nrm((N_ODD, d, N_EXPERTS), d ** -0.5),
        'od_exp_gate': nrm((N_ODD, N_EXPERTS, d, D_FF_EXPERT), d ** -0.5),
        'od_exp_up': nrm((N_ODD, N_EXPERTS, d, D_FF_EXPERT), d ** -0.5),
        'od_exp_down': nrm((N_ODD, N_EXPERTS, D_FF_EXPERT, d), D_FF_EXPERT ** -0.5),
    }


def reference(x, ev_norm1, ev_w_in, ev_qn_a, ev_kn_a, ev_qn_b, ev_kn_b, ev_sink_b, ev_w_out,
              ev_norm2, ev_ffn_gate, ev_ffn_up, ev_ffn_down,
              od_norm1, od_w_qkv, od_qn, od_kn, od_rpb, od_w_out, od_norm2, od_router,
              od_exp_gate, od_exp_up, od_exp_down):
    for layer in range(DEPTH):
        j = layer // 2
        if layer % 2 == 0:
            h = rms_norm(x, ev_norm1[j])
            x = x + even_mixer(h, ev_w_in[j], ev_qn_a[j], ev_kn_a[j], ev_qn_b[j], ev_kn_b[j],
                               ev_sink_b[j], ev_w_out[j])
            h = rms_norm(x, ev_norm2[j])
            x = x + swiglu(h, ev_ffn_gate[j], ev_ffn_up[j], ev_ffn_down[j])
        else:
            h = rms_norm(x, od_norm1[j])
            x = x + neighbourhood_mixer(h, od_w_qkv[j], od_qn[j], od_kn[j], od_rpb[j], od_w_out[j])
            h = rms_norm(x, od_norm2[j])
            x = x + moe_swiglu(h, od_router[j], od_exp_gate[j], od_exp_up[j], od_exp_down[j])
    return x
```

```python
import numpy as np
import ml_dtypes
from contextlib import ExitStack
import concourse.bass as bass
import concourse.mybir as mybir
from concourse.bass_utils import run_bass_kernel_spmd

F32 = mybir.dt.float32
BF16 = mybir.dt.bfloat16
ALU = mybir.AluOpType
AF = mybir.ActivationFunctionType

D = 1024
KC = 8
NOWN = 2048
NE = 2304
NK = 3328
FF0 = 2816
FF1 = 3584
NEXP = 8
EPS = 1e-6
DBG = {}


_UN = [0]


def _un(name):
    _UN[0] += 1
    return '%s_u%d' % (name, _UN[0])


class Sem:
    _n = [0]

    def __init__(self, h):
        self.h = h
        self.v = 0
        Sem._n[0] += 1
        self.uid = Sem._n[0]


class Sched:
    ENG = ('pe', 'act', 'dve', 'pool', 'sp')

    def __init__(self, nc, stack):
        self.nc = nc
        self.stack = stack
        self.n = 0
        self.sem = {e: self.new_sem('eng_' + e) for e in self.ENG}
        self.waited = {e: {} for e in self.ENG}
        self.ops = {e: [] for e in self.ENG}

    def new_sem(self, name):
        self.n += 1
        return Sem(self.stack.enter_context(self.nc.semaphore('%s_%d' % (name, self.n))))

    def op(self, eng, fn, deps=(), dma_sem=None, seq=False):
        own = self.sem[eng]
        waits = []
        flat = []
        for t in deps:
            if isinstance(t, list):
                flat.extend(t)
            else:
                flat.append(t)
        deps = flat
        if seq and own.v > 0:
            deps.append((own, own.v))
        for t in deps:
            if t is None:
                continue
            s, v = t
            if self.waited[eng].get(s.uid, 0) >= v:
                continue
            self.waited[eng][s.uid] = v
            waits.append((s, v))
        if dma_sem is None:
            own.v += 1
            ticket = (own, own.v)
            inc = (own, 1)
        else:
            dma_sem.v += 16
            ticket = (dma_sem, dma_sem.v)
            inc = (dma_sem, 16)
        self.ops[eng].append((fn, waits, inc))
        return ticket

    def emit(self, final_waits=()):
        nc = self.nc
        ops = self.ops

        def run(e, lst, extra=()):
            for fn, waits, inc in lst:
                for s, v in waits:
                    e.wait_ge(s.h, v)
                ins = fn(e)
                ins.then_inc(inc[0].h, inc[1])
            for s, v in extra:
                e.wait_ge(s.h, v)

        with nc.Block() as block:
            block.tensor(lambda e: run(e, ops['pe']))
            block.scalar(lambda e: run(e, ops['act']))
            block.vector(lambda e: run(e, ops['dve']))
            block.gpsimd(lambda e: run(e, ops['pool']))
            block.sync(lambda e: run(e, ops['sp'], final_waits))
        self.ops = {e: [] for e in self.ENG}


class Banks:
    def __init__(self, tiles):
        self.tiles = tiles
        self.rel = [None] * len(tiles)
        self.i = 0

    def get(self):
        b = self.i
        self.i = (self.i + 1) % len(self.tiles)
        return b, self.tiles[b], self.rel[b]

    def release(self, b, ticket):
        self.rel[b] = ticket


def groups_of(n, g=512):
    out = []
    s = 0
    while s < n:
        out.append((s, min(g, n - s)))
        s += g
    return out


def ffn_phase(nc, S, xin, xout, gain, wg, wu, wd, ff, parts, router=None, out_sem=None, pmax=1024, tg=512,
              oT=None, wout=None, hout=None, gain2=None):
    E = wg.shape[0]
    moe = router is not None
    PMAX = pmax
    with ExitStack() as st:
        def sb(name, shape, dt):
            return st.enter_context(nc.sbuf_tensor(_un(name), shape, dt))

        yacc = sb('yacc', [128, KC, PMAX], F32)
        hT = sb('hT', [128, KC, PMAX], BF16)
        sq = [sb('sq%d' % i, [128, KC, 512], BF16) for i in range(2)]
        rstd = sb('rstd', [128, PMAX], F32)
        gn = sb('gn', [128, KC], F32)
        ones = sb('ones', [128, 128], BF16)
        epsc = sb('epsc', [128, 1], F32)
        wgb = [sb('wgb%d' % i, [128, KC, 512], BF16) for i in range(2)]
        wub = [sb('wub%d' % i, [128, KC, 512], BF16) for i in range(2)]
        wdb = [sb('wdb%d' % i, [128, 4, D], BF16) for i in range(2)]
        actT = [sb('actT%d' % i, [128, 4, PMAX], BF16) for i in range(2)]
        sil = [sb('sil%d' % i, [128, 512], BF16) for i in range(4)]
        if hout is not None:
            gn2 = sb('gn2', [128, KC], F32)
        fuse_o = oT is not None
        if fuse_o:
            wo = sb('wo', [128, KC, D], BF16)
        if moe:
            cbc = sb('cbc', [128, E, PMAX], BF16)
            wr = sb('wr', [128, KC, E], F32)
            wrb = sb('wrb', [128, KC, E], BF16)
            ident = sb('ident', [128, 128], F32)
            onesf = sb('onesf', [128, 128], F32)
            lg = sb('lg', [128, 8, E], F32)
            eq1 = sb('eq1', [128, 8, E], F32)
            eq2 = sb('eq2', [128, 8, E], F32)
            msk = sb('msk', [128, 8, E], F32)
            comb = sb('comb', [128, 8, E], F32)
            m1t = sb('m1t', [128, 8], F32)
            m2t = sb('m2t', [128, 8], F32)
            dmt = sb('dmt', [128, 8], F32)
            ext = sb('ext', [128, 8], F32)
            g1t = sb('g1t', [128, 8], F32)
            g2t = sb('g2t', [128, 8], F32)
            rsT = sb('rsT', [128, 8], F32)
            inv128 = sb('inv128', [128, 1], F32)
            dg = [sb('dg%d' % i, [128, 4, 128], BF16) for i in range(4)]
            onesb = sb('onesb', [128, 128], BF16)
        ps = [st.enter_context(nc.psum_tensor(_un('ps%d' % i), [128, 512], F32)) for i in range(8)]
        banks = Banks(ps)
        sem_x = S.new_sem('ffn_x')
        sem_c = S.new_sem('ffn_c')
        sem_wa = [S.new_sem('ffn_wa') for _ in range(2)]
        sem_wd = [S.new_sem('ffn_wd') for _ in range(2)]
        sem_st = out_sem if out_sem is not None else S.new_sem('ffn_st')

        if fuse_o:
            t_wo = S.op('pool', lambda e: e.dma_start(out=wo[:], in_=wout.rearrange('(k q) d -> q k d', q=128)),
                        dma_sem=S.new_sem('ffn_wo'))
            sem_o = S.new_sem('ffn_o')
        t_g = S.op('sp', lambda e: e.dma_start(out=gn[:], in_=gain), dma_sem=sem_c)
        t_hs = None
        if hout is not None:
            t_g2 = S.op('sp', lambda e: e.dma_start(out=gn2[:], in_=gain2), dma_sem=S.new_sem('ffn_c3'))
            sem_hs = S.new_sem('ffn_hs')
        t_ones = S.op('pool', lambda e: e.memset(ones[:], 1.0 / D))
        t_eps = S.op('pool', lambda e: e.memset(epsc[:], EPS))
        if moe:
            t_wr = S.op('sp', lambda e: e.dma_start(out=wr[:], in_=router),
                        dma_sem=S.new_sem('ffn_c2'))
            t_c0 = S.op('pool', lambda e: e.memset(ident[:], 0.0))
            t_id = S.op('pool', lambda e: e.affine_select(out=ident[:], in_=ident[:], pattern=[[-1, 128]],
                                                          compare_op=ALU.not_equal, fill=1.0, base=0,
                                                          channel_multiplier=1), deps=[t_c0])
            S.op('pool', lambda e: e.memset(onesf[:], 1.0))
            S.op('pool', lambda e: e.memset(onesb[:], 1.0))
            t_of = S.op('pool', lambda e: e.memset(inv128[:], 1.0 / 128))
            t_wrs = S.op('dve', lambda e: e.tensor_copy(wrb[:], wr[:]), deps=[t_wr])

        blocks = []
        for ex in range(E):
            for (f0, fw) in groups_of(ff, 512):
                blocks.append((ex, f0, fw))
        nblk = len(blocks)

        last_store = None
        pe_a_done = {}
        pe_b_done = {}
        gi = 0
        last_part_reads = []
        for (t0, n) in parts:
            tgs = groups_of(n, tg)
            mm_hist = []
            t_ld = S.op('sp', lambda e, t0=t0, n=n: e.dma_start(
                out=yacc[:, :, 0:n], in_=xin[:, t0:t0 + n].rearrange('(k p) t -> p k t', p=128)),
                deps=last_part_reads, dma_sem=sem_x)
            t_res = {}
            if fuse_o:
                t_lo = S.op('sp', lambda e, t0=t0, n=n: e.dma_start(
                    out=hT[:, :, 0:n], in_=oT[:, t0:t0 + n].rearrange('(k p) t -> p k t', p=128)),
                    deps=last_part_reads, dma_sem=sem_o)
                for (g0, gw) in tgs:
                    t_a = None
                    for d in range(KC):
                        b, pt, rel = banks.get()
                        t_m = None
                        for k in range(KC):
                            t_m = S.op('pe', lambda e, k=k, d=d, g0=g0, gw=gw, pt=pt: e.matmul(
                                pt[:, 0:gw], wo[:, k, d * 128:(d + 1) * 128], hT[:, k, g0:g0 + gw],
                                start=(k == 0), stop=(k == KC - 1)), deps=[t_wo, t_lo, rel] + last_part_reads)
                        t_a = S.op('dve', lambda e, d=d, g0=g0, gw=gw, pt=pt: e.tensor_tensor(
                            yacc[:, d, g0:g0 + gw], pt[:, 0:gw], yacc[:, d, g0:g0 + gw], ALU.add),
                            deps=[t_m, t_ld], seq=True)
                        banks.release(b, t_a)
                    t_res[g0] = t_a
            t_h = []
            for gidx, (g0, gw) in enumerate(tgs):
                sqb = sq[gidx % 2]
                t_sq = None
                for k in range(KC):
                    t_sq = S.op('act', lambda e, k=k, g0=g0, gw=gw, sqb=sqb: e.activation(
                        out=sqb[:, k, 0:gw], in_=yacc[:, k, g0:g0 + gw], func=AF.Square),
                        deps=[t_ld, t_res.get(g0)] + last_part_reads + ([mm_hist[gidx - 2]] if gidx >= 2 else []))
                b, pt, rel = banks.get()
                t_mm = None
                for k in range(KC):
                    t_mm = S.op('pe', lambda e, k=k, gw=gw, sqb=sqb, pt=pt: e.matmul(
                        pt[:, 0:gw], ones[:], sqb[:, k, 0:gw], start=(k == 0), stop=(k == KC - 1)),
                        deps=[t_sq, rel, t_ones])
                mm_hist.append(t_mm)
                t_s = S.op('act', lambda e, g0=g0, gw=gw, pt=pt: e.activation(
                    out=rstd[:, g0:g0 + gw], in_=pt[:, 0:gw], func=AF.Ln, bias=epsc[:, 0:1], scale=1.0), deps=[t_mm, t_eps])
                banks.release(b, t_s)
                t_r = S.op('act', lambda e, g0=g0, gw=gw: e.activation(
                    out=rstd[:, g0:g0 + gw], in_=rstd[:, g0:g0 + gw], func=AF.Exp, scale=-0.5), deps=[t_s])
                for k in range(KC):
                    t_hk = S.op('dve', lambda e, k=k, g0=g0, gw=gw: e.scalar_tensor_tensor(
                        out=hT[:, k, g0:g0 + gw], in0=yacc[:, k, g0:g0 + gw], scalar=gn[:, k:k + 1],
                        in1=rstd[:, g0:g0 + gw], op0=ALU.mult, op1=ALU.mult),
                        deps=[t_g, t_ld, t_r, t_res.get(g0)] + last_part_reads)
                t_h.append(t_hk)
            t_hall = t_h[-1]
            t_cb = None
            if moe:
                ntile = n // 128
                t_l = None
                for ti in range(ntile):
                    c0 = ti * 128
                    b, pt, rel = banks.get()
                    t_mm = None
                    for k in range(KC):
                        t_mm = S.op('pe', lambda e, k=k, c0=c0, pt=pt: e.matmul(
                            pt[:, 0:E], hT[:, k, c0:c0 + 128], wrb[:, k, :], start=(k == 0), stop=(k == KC - 1)),
                            deps=[t_hall, t_wrs, rel])
                    t_l = S.op('dve', lambda e, ti=ti, pt=pt: e.tensor_copy(lg[:, ti, :], pt[:, 0:E]),
                               deps=[t_mm], seq=True)
                    banks.release(b, t_l)
                nt = ntile
                X = mybir.AxisListType.X
                S.op('dve', lambda e, nt=nt: e.tensor_reduce(out=m1t[:, 0:nt], in_=lg[:, 0:nt, :], axis=X, op=ALU.max), seq=True)
                S.op('dve', lambda e, nt=nt: e.tensor_tensor(eq1[:, 0:nt, :], lg[:, 0:nt, :],
                                                      m1t[:, 0:nt].unsqueeze(2).broadcast_to([128, nt, E]), ALU.is_equal), seq=True)
                S.op('dve', lambda e, nt=nt: e.scalar_tensor_tensor(out=msk[:, 0:nt, :], in0=eq1[:, 0:nt, :], scalar=-1e30,
                                                             in1=lg[:, 0:nt, :], op0=ALU.mult, op1=ALU.add), seq=True)
                S.op('dve', lambda e, nt=nt: e.tensor_reduce(out=m2t[:, 0:nt], in_=msk[:, 0:nt, :], axis=X, op=ALU.max), seq=True)
                S.op('dve', lambda e, nt=nt: e.tensor_tensor(eq2[:, 0:nt, :], msk[:, 0:nt, :],
                                                      m2t[:, 0:nt].unsqueeze(2).broadcast_to([128, nt, E]), ALU.is_equal), seq=True)
                t_dm = S.op('dve', lambda e, nt=nt: e.tensor_tensor(dmt[:, 0:nt], m2t[:, 0:nt], m1t[:, 0:nt], ALU.subtract), seq=True)
                t_ex = S.op('act', lambda e, nt=nt: e.activation(out=ext[:, 0:nt], in_=dmt[:, 0:nt], func=AF.Exp), deps=[t_dm])
                S.op('dve', lambda e, nt=nt: e.tensor_scalar(g1t[:, 0:nt], ext[:, 0:nt], 1.0, None, ALU.add), deps=[t_ex], seq=True)
                S.op('dve', lambda e, nt=nt: e.reciprocal(g1t[:, 0:nt], g1t[:, 0:nt]), seq=True)
                S.op('dve', lambda e, nt=nt: e.tensor_tensor(g2t[:, 0:nt], ext[:, 0:nt], g1t[:, 0:nt], ALU.mult), seq=True)
                S.op('dve', lambda e, nt=nt: e.tensor_tensor(eq1[:, 0:nt, :], eq1[:, 0:nt, :],
                                                      g1t[:, 0:nt].unsqueeze(2).broadcast_to([128, nt, E]), ALU.mult), seq=True)
                S.op('dve', lambda e, nt=nt: e.tensor_tensor(eq2[:, 0:nt, :], eq2[:, 0:nt, :],
                                                      g2t[:, 0:nt].unsqueeze(2).broadcast_to([128, nt, E]), ALU.mult), seq=True)
                S.op('dve', lambda e, nt=nt: e.tensor_tensor(comb[:, 0:nt, :], eq1[:, 0:nt, :], eq2[:, 0:nt, :], ALU.add), seq=True)
                if DBG:
                    t_cm = S.op('dve', lambda e, nt=nt: e.tensor_copy(comb[:, 0:nt, :], comb[:, 0:nt, :]), seq=True)
                    for nm, tt in (('ident', ident), ('lg', lg), ('comb', comb), ('eq1', eq1), ('m1t', m1t), ('g1t', g1t)):
                        if nm in DBG:
                            S.op('sp', lambda e, nm=nm, tt=tt: e.dma_start(out=DBG[nm], in_=tt[:]), deps=[t_cm, t_id],
                                 dma_sem=S.new_sem('dbg'))
                dg_rel = [None] * 4
                di = 0
                for ti in range(ntile):
                    c0 = ti * 128
                    for hf in range(E // 4):
                        dgb = dg[di % 4]
                        t_d = S.op('dve', lambda e, ti=ti, hf=hf, dgb=dgb: e.tensor_tensor(
                            dgb[:, :, :], ident[:, :].unsqueeze(1).broadcast_to([128, 4, 128]),
                            comb[:, ti, 4 * hf:4 * hf + 4].unsqueeze(2).broadcast_to([128, 4, 128]), ALU.mult),
                            deps=[dg_rel[di % 4], t_id])
                        b, pt, rel = banks.get()
                        t_bm = S.op('pe', lambda e, dgb=dgb, pt=pt: e.matmul(
                            pt[:, 0:512], onesb[:, :], dgb[:, :, :].rearrange('p e n -> p (e n)'),
                            start=True, stop=True), deps=[t_d, rel, t_of])
                        dg_rel[di % 4] = t_bm
                        di += 1
                        t_cb = S.op('act', lambda e, hf=hf, c0=c0, pt=pt: e.activation(
                            out=cbc[:, 4 * hf:4 * hf + 4, c0:c0 + 128],
                            in_=pt[:, 0:512].rearrange('p (e n) -> p e n', e=4), func=AF.Copy),
                            deps=[t_bm] + last_part_reads)
                        banks.release(b, t_cb)
            sil_rel = [None] * 4
            nb = nblk
            a_tickets = {}

            def pass_a(i):
                ex, f0, fw = blocks[i]
                g = gi + i
                par = g % 2
                dep_free = [pe_a_done.get(g - 2)]
                S.op('pool', lambda e: e.dma_start(
                    out=wgb[par][:, :, 0:fw], in_=wg[ex, :, f0:f0 + fw].rearrange('(k p) f -> p k f', p=128)),
                    deps=dep_free, dma_sem=sem_wa[par])
                t_w = S.op('pool', lambda e: e.dma_start(
                    out=wub[par][:, :, 0:fw], in_=wu[ex, :, f0:f0 + fw].rearrange('(k p) f -> p k f', p=128)),
                    deps=dep_free, dma_sem=sem_wa[par])
                t_wdl = S.op('pool', lambda e: e.dma_start(
                    out=wdb[par][:, 0:fw // 128, :], in_=wd[ex, f0:f0 + fw, :].rearrange('(c p) d -> p c d', p=128)),
                    deps=[pe_b_done.get(g - 2)], dma_sem=sem_wd[par])
                a_tickets[i] = t_wdl
                t_last = None
                t_act_last = None
                for c in range(fw // 128):
                    for (g0, gw) in tgs:
                        bg, pg, relg = banks.get()
                        for k in range(KC):
                            t_g1 = S.op('pe', lambda e, k=k, c=c, g0=g0, gw=gw, pg=pg: e.matmul(
                                pg[:, 0:gw], wgb[par][:, k, c * 128:(c + 1) * 128], hT[:, k, g0:g0 + gw],
                                start=(k == 0), stop=(k == KC - 1)), deps=[t_w, t_hall, relg])
                        bu, pu, relu = banks.get()
                        for k in range(KC):
                            t_u1 = S.op('pe', lambda e, k=k, c=c, g0=g0, gw=gw, pu=pu: e.matmul(
                                pu[:, 0:gw], wub[par][:, k, c * 128:(c + 1) * 128], hT[:, k, g0:g0 + gw],
                                start=(k == 0), stop=(k == KC - 1)), deps=[t_w, t_hall, relu])
                        t_last = t_u1
                        si = sil_rel.index(min(sil_rel, key=lambda t: -1 if t is None else t[1]))
                        sbuf_s = sil[si]
                        t_si = S.op('act', lambda e, gw=gw, pg=pg, sbuf_s=sbuf_s: e.activation(
                            out=sbuf_s[:, 0:gw], in_=pg[:, 0:gw], func=AF.Silu), deps=[t_g1, sil_rel[si]])
                        banks.release(bg, t_si)
                        if moe:
                            t_si = S.op('dve', lambda e, g0=g0, gw=gw, ex=ex, sbuf_s=sbuf_s: e.tensor_tensor(
                                sbuf_s[:, 0:gw], sbuf_s[:, 0:gw], cbc[:, ex, g0:g0 + gw], ALU.mult),
                                deps=[t_si, t_cb])
                        t_m = S.op('dve', lambda e, c=c, g0=g0, gw=gw, pu=pu, sbuf_s=sbuf_s: e.tensor_tensor(
                            actT[par][:, c, g0:g0 + gw], pu[:, 0:gw], sbuf_s[:, 0:gw], ALU.mult),
                            deps=[t_u1, t_si, pe_b_done.get(g - 2)])
                        sil_rel[si] = t_m
                        banks.release(bu, t_m)
                        t_act_last = t_m
                pe_a_done[g] = t_last
                return t_act_last

            def pass_b(i, t_act, last=False):
                ex, f0, fw = blocks[i]
                g = gi + i
                par = g % 2
                nc_ = fw // 128
                t_last = None
                t_acc = None
                for d in range(KC):
                    for (g0, gw) in tgs:
                        by, py, rely = banks.get()
                        for c in range(nc_):
                            t_y = S.op('pe', lambda e, c=c, d=d, g0=g0, gw=gw, py=py: e.matmul(
                                py[:, 0:gw], wdb[par][:, c, d * 128:(d + 1) * 128], actT[par][:, c, g0:g0 + gw],
                                start=(c == 0), stop=(c == nc_ - 1)), deps=[a_tickets[i], t_act, rely])
                        t_last = t_y
                        t_acc = S.op('dve', lambda e, d=d, g0=g0, gw=gw, py=py: e.tensor_tensor(
                            yacc[:, d, g0:g0 + gw], py[:, 0:gw], yacc[:, d, g0:g0 + gw], ALU.add), deps=[t_y])
                        banks.release(by, t_acc)
                    if last:
                        st_box[0] = S.op('sp', lambda e, d=d, t0=t0, n=n: e.dma_start(
                            out=xout[d * 128:(d + 1) * 128, t0:t0 + n], in_=yacc[:, d, 0:n]),
                            deps=[t_acc], dma_sem=sem_st)
                pe_b_done[g] = t_last
                return t_acc

            t_acts = {}
            t_acc_last = None
            st_box = [None]
            t_acts[0] = pass_a(0)
            for i in range(nb):
                if i + 1 < nb:
                    t_acts[i + 1] = pass_a(i + 1)
                t_acc_last = pass_b(i, t_acts[i], last=(i == nb - 1))
            gi += nb
            last_store = st_box[0]
            last_part_reads = [last_store, pe_a_done[gi - 1], pe_b_done[gi - 1]]
            if hout is not None:
                t_hk = None
                for gidx, (g0, gw) in enumerate(tgs):
                    sqb = sq[gidx % 2]
                    t_sq = None
                    for k in range(KC):
                        t_sq = S.op('act', lambda e, k=k, g0=g0, gw=gw, sqb=sqb: e.activation(
                            out=sqb[:, k, 0:gw], in_=yacc[:, k, g0:g0 + gw], func=AF.Square),
                            deps=[t_acc_last] + mm_hist[-2:])
                    b, pt, rel = banks.get()
                    t_mm = None
                    for k in range(KC):
                        t_mm = S.op('pe', lambda e, k=k, gw=gw, sqb=sqb, pt=pt: e.matmul(
                            pt[:, 0:gw], ones[:], sqb[:, k, 0:gw], start=(k == 0), stop=(k == KC - 1)),
                            deps=[t_sq, rel, t_ones])
                    mm_hist.append(t_mm)
                    t_s = S.op('act', lambda e, g0=g0, gw=gw, pt=pt: e.activation(
                        out=rstd[:, g0:g0 + gw], in_=pt[:, 0:gw], func=AF.Ln, bias=epsc[:, 0:1], scale=1.0),
                        deps=[t_mm, t_eps, t_hall])
                    banks.release(b, t_s)
                    t_r = S.op('act', lambda e, g0=g0, gw=gw: e.activation(
                        out=rstd[:, g0:g0 + gw], in_=rstd[:, g0:g0 + gw], func=AF.Exp, scale=-0.5), deps=[t_s])
                    for k in range(KC):
                        t_hk = S.op('dve', lambda e, k=k, g0=g0, gw=gw: e.scalar_tensor_tensor(
                            out=hT[:, k, g0:g0 + gw], in0=yacc[:, k, g0:g0 + gw], scalar=gn2[:, k:k + 1],
                            in1=rstd[:, g0:g0 + gw], op0=ALU.mult, op1=ALU.mult),
                            deps=[t_g2, t_r, t_acc_last, pe_a_done[gi - 1]])
                t_hs = S.op('sp', lambda e, t0=t0, n=n: e.dma_start(
                    out=hout[:, t0:t0 + n].rearrange('(k p) t -> p k t', p=128), in_=hT[:, :, 0:n]),
                    deps=[t_hk], dma_sem=sem_hs)
                last_part_reads = last_part_reads + [t_hs, t_hk]
        S.emit(final_waits=[last_store] + ([t_hs] if t_hs is not None else []))
    return last_store


def norm_phase(nc, S, xin, hout, gain, n):
    with ExitStack() as st:
        def sb(name, shape, dt):
            return st.enter_context(nc.sbuf_tensor(_un(name), shape, dt))
        NB = 4
        xb = [sb('nx%d' % i, [128, KC, 512], F32) for i in range(NB)]
        hb = [sb('nh%d' % i, [128, KC, 512], BF16) for i in range(NB)]
        sq = [sb('nsq%d' % i, [128, KC, 512], BF16) for i in range(NB)]
        rs = [sb('nrs%d' % i, [128, 512], F32) for i in range(NB)]
        gn = sb('ngn', [128, KC], F32)
        ones = sb('nones', [128, 128], BF16)
        epsc = sb('nepsc', [128, 1], F32)
        ps = [st.enter_context(nc.psum_tensor(_un('nps%d' % i), [128, 512], F32)) for i in range(NB)]
        t_g = S.op('sp', lambda e: e.dma_start(out=gn[:], in_=gain), dma_sem=S.new_sem('n_g'))
        S.op('pool', lambda e: e.memset(ones[:], 1.0 / D))
        t_o = S.op('pool', lambda e: e.memset(epsc[:], EPS))
        sx = [S.new_sem('n_x') for _ in range(NB)]
        so = [S.new_sem('n_o') for _ in range(NB)]
        prev = [None] * NB
        last = None
        for gi, (g0, gw) in enumerate(groups_of(n, 512)):
            p = gi % NB
            t_ld = S.op('sp', lambda e, g0=g0, gw=gw, p=p: e.dma_start(
                out=xb[p][:, :, 0:gw], in_=xin[:, g0:g0 + gw].rearrange('(k q) t -> q k t', q=128)),
                deps=[prev[p]], dma_sem=sx[p])
            t_sq = None
            for k in range(KC):
                t_sq = S.op('act', lambda e, k=k, gw=gw, p=p: e.activation(
                    out=sq[p][:, k, 0:gw], in_=xb[p][:, k, 0:gw], func=AF.Square), deps=[t_ld, prev[p]])
            for k in range(KC):
                t_mm = S.op('pe', lambda e, k=k, gw=gw, p=p: e.matmul(
                    ps[p][:, 0:gw], ones[:], sq[p][:, k, 0:gw], start=(k == 0), stop=(k == KC - 1)),
                    deps=[t_sq, t_o, prev[p]])
            t_s = S.op('act', lambda e, gw=gw, p=p: e.activation(
                out=rs[p][:, 0:gw], in_=ps[p][:, 0:gw], func=AF.Ln, bias=epsc[:, 0:1], scale=1.0), deps=[t_mm, t_o])
            t_r = S.op('act', lambda e, gw=gw, p=p: e.activation(
                out=rs[p][:, 0:gw], in_=rs[p][:, 0:gw], func=AF.Exp, scale=-0.5), deps=[t_s])
            for k in range(KC):
                t_h = S.op('dve', lambda e, k=k, gw=gw, p=p: e.scalar_tensor_tensor(
                    out=hb[p][:, k, 0:gw], in0=xb[p][:, k, 0:gw], scalar=gn[:, k:k + 1], in1=rs[p][:, 0:gw],
                    op0=ALU.mult, op1=ALU.mult), deps=[t_g, t_r, prev[p]])
            last = S.op('pool', lambda e, g0=g0, gw=gw, p=p: e.dma_start(
                out=hout[:, g0:g0 + gw].rearrange('(k q) t -> q k t', q=128), in_=hb[p][:, :, 0:gw]),
                deps=[t_h], dma_sem=so[p])
            prev[p] = last
        S.emit(final_waits=[t for t in prev if t is not None])


def attn_phase(nc, S, hsrc, nk, nq, w, groups, oT, masks_dram, M):
    with ExitStack() as st:
        def sb(name, shape, dt):
            return st.enter_context(nc.sbuf_tensor(_un(name), shape, dt))
        maxkt = max(len(g['ktiles']) for g in groups)
        maxsl = max(len(g['wv']) for g in groups)
        hTs = sb('a_h', [128, KC, nk], BF16)
        va = sb('a_va', [128, maxkt, maxsl * 64], BF16)
        wva = sb('a_wv', [128, KC, maxsl * 64], BF16)
        wq = [sb('a_wq%d' % i, [128, KC, 128], BF16) for i in range(2)]
        wk = [sb('a_wk%d' % i, [128, KC, 128], BF16) for i in range(2)]
        qAB = [sb('a_qAB%d' % i, [128, 2, nq], BF16) for i in range(2)]
        kT = [sb('a_kT%d' % i, [128, nk], BF16) for i in range(2)]
        onum = sb('a_on', [128, nq], F32)
        oden = sb('a_od', [128, nq], F32)
        osb = sb('a_osb', [128, nq], BF16)
        mb = sb('a_mb', [128, 2, M], F32)
        mk = [sb('a_mk%d' % i, [128, 2, M], BF16) for i in range(2)]
        sqb = [sb('a_sq%d' % i, [128, 256], BF16) for i in range(3)]
        rsb = [sb('a_rs%d' % i, [128, 256], F32) for i in range(3)]
        gq = [sb('a_gq%d' % i, [128, 1], F32) for i in range(2)]
        gk = [sb('a_gk%d' % i, [128, 1], F32) for i in range(2)]
        esk = [sb('a_esk%d' % i, [128, 1], F32) for i in range(2)]
        bd = sb('a_bd', [128, 128], BF16)
        onesb = sb('a_ones', [128, 128], BF16)
        epsc = sb('a_epsc', [128, 1], F32)
        pbuf = [sb('a_p%d' % i, [128, 512], BF16) for i in range(6)]
        psS = [st.enter_context(nc.psum_tensor(_un('a_ps%d' % i), [128, 512], F32)) for i in range(2)]
        psO = [st.enter_context(nc.psum_tensor(_un('a_po%d' % i), [128, 512], F32)) for i in range(2)]
        psD = [st.enter_context(nc.psum_tensor(_un('a_pd%d' % i), [128, 512], F32)) for i in range(2)]
        psP = [st.enter_context(nc.psum_tensor(_un('a_pp%d' % i), [128, 512], F32)) for i in range(2)]
        t_h = S.op('sp', lambda e: e.dma_start(
            out=hTs[:], in_=hsrc[:, 0:nk].rearrange('(k q) t -> q k t', q=128)), dma_sem=S.new_sem('a_h'))
        S.op('pool', lambda e: e.memset(bd[:], 0.0))
        S.op('pool', lambda e: e.memset(bd[0:64, 0:64], 1.0 / 64), seq=True)
        S.op('pool', lambda e: e.memset(bd[64:128, 64:128], 1.0 / 64), seq=True)
        S.op('pool', lambda e: e.memset(onesb[:], 1.0))
        S.op('pool', lambda e: e.memset(epsc[:], EPS))
        for i in range(2):
            S.op('pool', lambda e, i=i: e.memset(qAB[i][64:128, 0, :], 0.0))
            S.op('pool', lambda e, i=i: e.memset(qAB[i][0:64, 1, :], 0.0))
        t_c = S.op('pool', lambda e: e.memset(epsc[:], EPS), seq=True)
        sem_w = [S.new_sem('a_w') for _ in range(2)]
        sem_g = [S.new_sem('a_g') for _ in range(2)]
        sem_mb = S.new_sem('a_mb')
        sem_sk = [S.new_sem('a_sk') for _ in range(2)]
        sem_wv = S.new_sem('a_wv')
        sem_o = S.new_sem('a_o')
        brel = [None] * 4
        psALL = psS + psP
        pn = {'i': 0, 'sq': 0, 'sqrel': [None] * 3}
        ring = {'S': 0, 'O': 0, 'Orel': [None] * 2, 'P': 0, 'Prel': [None] * 6}
        fin_hist = []
        t_store = None
        t_osb_free = None
        t_acc_free = None
        t_mb_free = None
        gdone = None
        pi_glob = 0
        prev_tq = None

        PENDING = 'PENDING'
        NIDLE = 8
        G = {'t_mb_free': None, 'prev_tq': None, 't_wv': None}
        fin_by_idx = {}

        def proj_pair(wt, gt, n, t_w, t_gl, dsts, extra, out):
            last = None
            for (g0, gw) in groups_of(n, 256):
                r = pn['i'] % 2
                pn['i'] += 1
                q = pn['sq'] % 3
                pn['sq'] += 1
                pp = psP[r]
                rel = brel[2 + r]
                srel = pn['sqrel'][q]
                for k in range(KC):
                    t1 = S.op('pe', lambda e, k=k, g0=g0, gw=gw, pp=pp: e.matmul(
                        pp[:, 0:gw], wt[:, k, :], hTs[:, k, g0:g0 + gw], start=(k == 0), stop=(k == KC - 1)),
                        deps=[t_w, t_h, rel] + extra)
                t2 = S.op('act', lambda e, gw=gw, pp=pp, q=q: e.activation(
                    out=sqb[q][:, 0:gw], in_=pp[:, 0:gw], func=AF.Square), deps=[t1, srel])
                brel[2 + r] = PENDING
                yield
                t3 = S.op('pe', lambda e, gw=gw, pp=pp, q=q: e.matmul(
                    pp[:, 256:256 + gw], bd[:], sqb[q][:, 0:gw], start=True, stop=True), deps=[t2, t_c])
                t4 = S.op('act', lambda e, gw=gw, pp=pp, q=q: e.activation(
                    out=rsb[q][:, 0:gw], in_=pp[:, 256:256 + gw], func=AF.Ln, bias=epsc[:, 0:1], scale=1.0),
                    deps=[t3, srel, t_c])
                t5 = S.op('act', lambda e, gw=gw, q=q: e.activation(
                    out=rsb[q][:, 0:gw], in_=rsb[q][:, 0:gw], func=AF.Exp, scale=-0.5), deps=[t4])
                for (dst, r0, r1) in dsts:
                    last = S.op('dve', lambda e, g0=g0, gw=gw, pp=pp, q=q, dst=dst, r0=r0, r1=r1:
                                e.scalar_tensor_tensor(out=dst[r0:r1, g0:g0 + gw], in0=pp[r0:r1, 0:gw],
                                                       scalar=gt[r0:r1, 0:1], in1=rsb[q][r0:r1, 0:gw],
                                                       op0=ALU.mult, op1=ALU.mult), deps=[t5, t_gl, t1] + extra)
                brel[2 + r] = last
                pn['sqrel'][q] = last
                yield
            out.append(last)

        def prep(pr, pidx, stt):
            b2 = pidx % 2
            free2 = fin_by_idx.get(pidx - 2)
            thr = [free2, G['t_wv'], G['prev_tq']]
            S.op('pool', lambda e, pr=pr, b2=b2: e.dma_start(
                out=wq[b2][:], in_=w[:, pr['wq']:pr['wq'] + 128].rearrange('(k q) c -> q k c', q=128)),
                deps=thr, dma_sem=sem_w[b2])
            for hh in range(2):
                S.op('pool', lambda e, pr=pr, b2=b2, hh=hh: e.dma_start(
                    out=wk[b2][:, :, hh * 64:(hh + 1) * 64],
                    in_=w[:, pr['wk'][hh]:pr['wk'][hh] + 64].rearrange('(k q) c -> q k c', q=128)),
                    deps=thr, dma_sem=sem_w[b2])
            t_w = (sem_w[b2], sem_w[b2].v)
            S.op('sp', lambda e, pr=pr, b2=b2: e.dma_start(out=gq[b2][:], in_=pr['gq']), deps=[free2],
                 dma_sem=sem_g[b2])
            S.op('sp', lambda e, pr=pr, b2=b2: e.dma_start(out=gk[b2][:], in_=pr['gk']), deps=[free2],
                 dma_sem=sem_g[b2])
            t_gl = (sem_g[b2], sem_g[b2].v)
            t_ml = S.op('sp', lambda e, pr=pr: e.dma_start(
                out=mb[:], in_=masks_dram[pr['mi']:pr['mi'] + 2].rearrange('h p m -> p h m')),
                deps=[G['t_mb_free']], dma_sem=sem_mb)
            t_sk = None
            if pr.get('sink') is not None:
                t_sk = S.op('sp', lambda e, pr=pr, b2=b2: e.dma_start(out=esk[b2][:], in_=pr['sink']),
                            deps=[free2], dma_sem=sem_sk[b2])
            for _ in range(NIDLE + 1):
                yield
            t_mk = S.op('act', lambda e, b2=b2: e.activation(out=mk[b2][:], in_=mb[:], func=AF.Exp),
                        deps=[t_ml, free2])
            G['t_mb_free'] = t_mk
            if t_sk is not None:
                t_sk = S.op('act', lambda e, b2=b2: e.activation(out=esk[b2][:], in_=esk[b2][:], func=AF.Exp),
                            deps=[t_sk])
            oq, ok = [], []
            yield from proj_pair(wq[b2], gq[b2], nq, t_w, t_gl,
                                 [(qAB[b2][:, 0, :], 0, 64), (qAB[b2][:, 1, :], 64, 128)], [free2], oq)
            yield from proj_pair(wk[b2], gk[b2], nk, t_w, t_gl, [(kT[b2], 0, 128)], [free2], ok)
            G['prev_tq'] = oq[0]
            stt.update(b2=b2, t_q=oq[0], t_k=ok[0], t_mk=t_mk, t_sk=t_sk)

        flat = [(grp, pr) for grp in groups for pr in grp['pairs']]
        nsteps = 1 + NIDLE + 2 * (len(groups_of(nq, 256)) + len(groups_of(nk, 256)))
        cur_stt = {}
        for _ in prep(flat[0][1], 0, cur_stt):
            pass
        cur_grp = None
        for pidx, (grp, pr) in enumerate(flat):
            if grp is not cur_grp:
                cur_grp = grp
                kts = grp['ktiles']
                nsl = len(grp['wv'])
                t_wv = None
                for si, c0 in enumerate(grp['wv']):
                    t_wv = S.op('pool', lambda e, si=si, c0=c0: e.dma_start(
                        out=wva[:, :, si * 64:(si + 1) * 64], in_=w[:, c0:c0 + 64].rearrange('(k q) c -> q k c', q=128)),
                        deps=[gdone], dma_sem=sem_wv)
                G['t_wv'] = t_wv
                t_v = None
                for ti, (s0, stp, cnt) in enumerate(kts):
                    for c0 in range(0, nsl * 64, 512):
                        cw = min(512, nsl * 64 - c0)
                        r = pn['i'] % 2
                        pn['i'] += 1
                        pp = psP[r]
                        rel = brel[2 + r]
                        assert rel is not PENDING
                        for k in range(KC):
                            t1 = S.op('pe', lambda e, k=k, s0=s0, stp=stp, cnt=cnt, pp=pp, c0=c0, cw=cw: e.matmul(
                                pp[0:cnt, 0:cw], hTs[:, k, s0:s0 + stp * (cnt - 1) + 1:stp], wva[:, k, c0:c0 + cw],
                                start=(k == 0), stop=(k == KC - 1)), deps=[t_wv, t_h, rel, gdone])
                        t_v = S.op('act', lambda e, ti=ti, cnt=cnt, pp=pp, c0=c0, cw=cw: e.activation(
                            out=va[0:cnt, ti, c0:c0 + cw], in_=pp[0:cnt, 0:cw], func=AF.Copy), deps=[t1, gdone])
                        brel[2 + r] = t_v
            if True:
                stt = cur_stt
                b2, t_q, t_k, t_mk, t_sk = stt['b2'], stt['t_q'], stt['t_k'], stt['t_mk'], stt['t_sk']
                nxt, nxt_stt = None, {}
                if pidx + 1 < len(flat):
                    nxt = prep(flat[pidx + 1][1], pidx + 1, nxt_stt)
                vs = pr['vslot'] * 64
                seq = []
                for (qs, qstp, qn, klist, first) in pr['qtiles']:
                    qsl = slice(qs, qs + qstp * (qn - 1) + 1, qstp)
                    nkt = len(klist)
                    qt = dict(qsl=qsl, qn=qn, nkt=nkt, first=first)
                    for i in range(0, nkt, 2):
                        seq.append(dict(qt=qt, ch=klist[i:i + 2], j0=i, lastc=(i + 2 >= nkt)))

                def emit_scores(c):
                    qt = c['qt']
                    qsl, qn = qt['qsl'], qt['qn']
                    ch = c['ch']
                    while True:
                        si = ring['S'] % 4
                        ring['S'] += 1
                        if brel[si] is not PENDING:
                            break
                    pS = psALL[si]
                    srel = brel[si]
                    pi_ = ring['P'] % 6
                    ring['P'] += 1
                    pb = pbuf[pi_]
                    prel = ring['Prel'][pi_]
                    nch = len(ch)
                    t_s = None
                    for jj, (kt, moff) in enumerate(ch):
                        ks0, kstp, kcnt = kts[kt]
                        ksl = slice(ks0, ks0 + kstp * (kcnt - 1) + 1, kstp)
                        t_s = S.op('pe', lambda e, jj=jj, ksl=ksl, kcnt=kcnt, qsl=qsl, qn=qn, pS=pS, b2=b2: e.matmul(
                            pS[0:kcnt, :].rearrange('p (h c) -> p h c', h=2)[:, :, jj * 128:jj * 128 + qn],
                            kT[b2][:, ksl], qAB[b2][:, :, qsl], start=True, stop=True), deps=[t_q, t_k, srel])
                    t_e = S.op('act', lambda e, pS=pS, pb=pb, nch=nch: e.activation(
                        out=pb[:, :].rearrange('p (h c) -> p h c', h=2)[:, :, 0:nch * 128],
                        in_=pS[:, :].rearrange('p (h c) -> p h c', h=2)[:, :, 0:nch * 128],
                        func=AF.Exp, scale=0.125), deps=[t_s, prel])
                    brel[si] = t_e
                    contig = (nch == 1 or ch[1][1] == ch[0][1] + 128)
                    t_m = None
                    for hh in range(2):
                        if contig:
                            m0_ = ch[0][1]
                            wd_ = (nch - 1) * 128 + qn
                            t_m = S.op('dve', lambda e, hh=hh, m0_=m0_, wd_=wd_, pb=pb, b2=b2: e.tensor_tensor(
                                pb[:, hh * 256:hh * 256 + wd_], pb[:, hh * 256:hh * 256 + wd_],
                                mk[b2][:, hh, m0_:m0_ + wd_], ALU.mult), deps=[t_e, t_mk])
                        else:
                            for jj, (kt, moff) in enumerate(ch):
                                t_m = S.op('dve', lambda e, hh=hh, jj=jj, moff=moff, pb=pb, b2=b2, qn=qn: e.tensor_tensor(
                                    pb[:, hh * 256 + jj * 128:hh * 256 + jj * 128 + qn],
                                    pb[:, hh * 256 + jj * 128:hh * 256 + jj * 128 + qn],
                                    mk[b2][:, hh, moff:moff + qn], ALU.mult), deps=[t_e, t_mk])
                    c['pb'] = pb
                    c['pi'] = pi_
                    c['t_m'] = t_m

                def emit_pv(c):
                    qt = c['qt']
                    qsl, qn, nkt, first = qt['qsl'], qt['qn'], qt['nkt'], qt['first']
                    if c['j0'] == 0:
                        oi = ring['O'] % 2
                        ring['O'] += 1
                        qt['oi'] = oi
                        qt['orel'] = ring['Orel'][oi]
                    oi = qt['oi']
                    orel = qt['orel']
                    pO = psO[oi]
                    pD = psD[oi]
                    pb = c['pb']
                    t_m = c['t_m']
                    t_pv = None
                    for jj, (kt, moff) in enumerate(c['ch']):
                        kcnt = kts[kt][2]
                        jdone = c['j0'] + jj
                        st_ = (jdone == 0)
                        sp_ = (jdone == nkt - 1)
                        rhs = lambda pb=pb, jj=jj, kcnt=kcnt, qn=qn: pb[0:kcnt, :].rearrange(
                            'p (h c) -> p h c', h=2)[:, :, jj * 128:jj * 128 + qn]
                        S.op('pe', lambda e, kt=kt, kcnt=kcnt, qn=qn, pO=pO, rhs=rhs, st_=st_, sp_=sp_, vs=vs: e.matmul(
                            pO[:, 0:256].rearrange('p (h c) -> p h c', h=2)[:, :, 0:qn],
                            va[0:kcnt, kt, vs:vs + 128], rhs(), start=st_, stop=sp_),
                            deps=[t_m, t_v, orel])
                        t_pv = S.op('pe', lambda e, kcnt=kcnt, qn=qn, pD=pD, rhs=rhs, st_=st_, sp_=sp_: e.matmul(
                            pD[:, 0:256].rearrange('p (h c) -> p h c', h=2)[:, :, 0:qn],
                            onesb[0:kcnt, :], rhs(), start=st_, stop=sp_), deps=[t_m, orel])
                    ring['Prel'][c['pi']] = t_pv
                    if not c['lastc']:
                        return
                    evs = []
                    for (dst, pX) in ((onum, pO), (oden, pD)):
                        for hh in range(2):
                            r0, r1 = hh * 64, hh * 64 + 64
                            src = pX[r0:r1, hh * 128:hh * 128 + qn]
                            osl = dst[r0:r1, qsl]
                            if first:
                                if hh == 0:
                                    tk = S.op('act', lambda e, osl=osl, src=src: e.activation(out=osl, in_=src,
                                                                                              func=AF.Copy),
                                              deps=[t_pv, t_acc_free])
                                else:
                                    tk = S.op('dve', lambda e, osl=osl, src=src: e.tensor_copy(osl, src),
                                              deps=[t_pv, t_acc_free])
                                firsts[tk[0].uid] = tk
                            else:
                                tk = S.op('dve', lambda e, osl=osl, src=src: e.tensor_tensor(osl, src, osl, ALU.add),
                                          deps=[t_pv] + list(firsts.values()))
                                adds.append(tk)
                            evs.append(tk)
                    ring['Orel'][oi] = evs

                firsts = {}
                adds = []
                LA = 3
                per = -(-nsteps // max(1, len(seq) - LA - 2))
                for i_ in range(min(LA, len(seq))):
                    emit_scores(seq[i_])
                for i_, c in enumerate(seq):
                    if i_ + LA < len(seq):
                        emit_scores(seq[i_ + LA])
                    emit_pv(c)
                    if nxt is not None:
                        for _ in range(per):
                            if next(nxt, PENDING) is PENDING:
                                nxt = None
                                break
                if nxt is not None:
                    for _ in nxt:
                        pass
                last_add = adds[-1] if adds else None
                fin = list(firsts.values()) + [last_add]
                if t_sk is not None:
                    t_x = S.op('dve', lambda e, b2=b2: e.tensor_scalar(oden[:, :], oden[:, :], esk[b2][:, 0:1], None,
                                                                       ALU.add), deps=fin + [t_sk], seq=True)
                    fin = fin + [t_x]
                t_rd = S.op('act', lambda e: e.activation(out=oden[:, :], in_=oden[:, :], func=AF.Ln), deps=fin)
                t_rd = S.op('act', lambda e: e.activation(out=oden[:, :], in_=oden[:, :], func=AF.Exp, scale=-1.0),
                            deps=[t_rd])
                t_n = S.op('dve', lambda e: e.tensor_tensor(osb[:, :], onum[:, :], oden[:, :], ALU.mult),
                           deps=[t_rd, t_store] + fin)
                t_acc_free = t_n
                t_store = S.op('sp', lambda e, pr=pr: e.dma_start(out=oT[pr['orow']:pr['orow'] + 128, :], in_=osb[:, :]),
                               deps=[t_n], dma_sem=sem_o)
                fin_by_idx[pidx] = t_n
                gdone = t_n
                cur_stt = nxt_stt
        S.emit(final_waits=[t_store])


def oproj_phase(nc, S, oT, wout, xin, xout, n):
    with ExitStack() as st:
        def sb(name, shape, dt):
            return st.enter_context(nc.sbuf_tensor(_un(name), shape, dt))
        wo = sb('o_w', [128, KC, D], BF16)
        NB = 4
        ob = [sb('o_o%d' % i, [128, KC, 512], BF16) for i in range(NB)]
        xb = [sb('o_x%d' % i, [128, KC, 512], F32) for i in range(NB)]
        ps = [st.enter_context(nc.psum_tensor(_un('o_ps%d' % i), [128, 512], F32)) for i in range(4)]
        banks = Banks(ps)
        t_w = S.op('pool', lambda e: e.dma_start(out=wo[:], in_=wout.rearrange('(k q) d -> q k d', q=128)),
                   dma_sem=S.new_sem('o_w'))
        sl = [S.new_sem('o_l') for _ in range(NB)]
        ss = [S.new_sem('o_s') for _ in range(NB)]
        prev = [None] * NB
        for gi, (g0, gw) in enumerate(groups_of(n, 512)):
            p = gi % NB
            S.op('sp', lambda e, g0=g0, gw=gw, p=p: e.dma_start(
                out=ob[p][:, :, 0:gw], in_=oT[:, g0:g0 + gw].rearrange('(k q) t -> q k t', q=128)),
                deps=[prev[p]], dma_sem=sl[p])
            t_l = S.op('sp', lambda e, g0=g0, gw=gw, p=p: e.dma_start(
                out=xb[p][:, :, 0:gw], in_=xin[:, g0:g0 + gw].rearrange('(k q) t -> q k t', q=128)),
                deps=[prev[p]], dma_sem=sl[p])
            t_a = None
            for d in range(KC):
                b, pt, rel = banks.get()
                for k in range(KC):
                    t_m = S.op('pe', lambda e, k=k, d=d, gw=gw, p=p, pt=pt: e.matmul(
                        pt[:, 0:gw], wo[:, k, d * 128:(d + 1) * 128], ob[p][:, k, 0:gw],
                        start=(k == 0), stop=(k == KC - 1)), deps=[t_w, t_l, rel])
                t_a = S.op('dve', lambda e, d=d, gw=gw, p=p, pt=pt: e.tensor_tensor(
                    xb[p][:, d, 0:gw], pt[:, 0:gw], xb[p][:, d, 0:gw], ALU.add), deps=[t_m, t_l], seq=True)
                banks.release(b, t_a)
            prev[p] = S.op('act', lambda e, g0=g0, gw=gw, p=p: e.dma_start(
                out=xout[:, g0:g0 + gw].rearrange('(k q) t -> q k t', q=128), in_=xb[p][:, :, 0:gw]),
                deps=[t_a], dma_sem=ss[p])
        S.emit(final_waits=[t for t in prev if t is not None])


NEGB = -30000.0
_DEBUG_HOOK = []
DIL = (1, 4, 16)


def _alibi():
    s = np.exp2(-8.0 * np.arange(1, 17) / 16).astype(np.float32)
    return s[0::2], s[1::2]


def _heads_layer0(qn_a, kn_a, qn_b, kn_b, sinkp):
    ktiles = []
    tid = {}
    for ci, r in enumerate(DIL):
        lk = NE // r + 64
        for p in range(r):
            for j, (l0, cnt) in enumerate(groups_of(lk, 128)):
                tid[(ci, p, j)] = len(ktiles)
                ktiles.append((l0 * r + p, r, cnt))
    qtiles = []
    for ci, r in enumerate(DIL):
        lq = NE // r
        lo, hi = ci * 256, ci * 256 + 128
        for p in range(r):
            qtiles.append((p, r, 64, [(tid[(ci, p, 0)], hi + 64)], ci == 0))
            m = 0
            l0 = 64
            while l0 < lq:
                qn = min(128, lq - l0)
                qtiles.append((l0 * r + p, r, qn, [(tid[(ci, p, m)], lo), (tid[(ci, p, m + 1)], hi)], ci == 0))
                l0 += 128
                m += 1
    gas = [dict(wv=[1024 + 64 * h for h in range(4 * g, 4 * g + 4)], ktiles=ktiles, pairs=[
        dict(wq=128 * p, wk=(512 + 128 * p, 512 + 128 * p + 64), vslot=2 * (p % 2), gq=qn_a, gk=kn_a, sink=None,
             orow=128 * p, mi=2 * p, qtiles=qtiles) for p in (2 * g, 2 * g + 1)]) for g in range(2)]
    ktb = [(128 * j, 1, 128) for j in range(NE // 128 + 1)]
    qtb = []
    for m in range(NE // 128):
        kl = []
        if m > 0:
            kl.append((m - 1, 0))
        kl.append((m, 128))
        kl.append((m + 1, 256))
        qtb.append((128 * m, 1, 128, kl, True))
    gb = dict(wv=[2176, 2176, 2240, 2240], ktiles=ktb, pairs=[
        dict(wq=1536 + 128 * p, wk=(2048 + 64 * (p // 2), 2048 + 64 * (p // 2)), vslot=2 * (p // 2), gq=qn_b, gk=kn_b,
             sink=sinkp[p], orow=512 + 128 * p, mi=8 + 2 * p, qtiles=qtb) for p in range(4)])
    return gas + [gb]


def _masks_layer0():
    sa, sb_ = _alibi()
    kk = np.arange(128)[:, None].astype(np.float32)
    qq = np.arange(128)[None, :].astype(np.float32)
    out = np.full((16, 128, 768), NEGB, np.float32)
    for h in range(8):
        for ci, r in enumerate(DIL):
            d = 64 + qq - kk
            out[h, :, ci * 256:ci * 256 + 128] = np.where(np.abs(d) <= 64, -sa[h] * r * np.abs(d), NEGB)
            d = qq - kk - 64
            out[h, :, ci * 256 + 128:ci * 256 + 256] = np.where(np.abs(d) <= 64, -sa[h] * r * np.abs(d), NEGB)
    for h in range(8):
        for j, off in enumerate((128, 0, -128)):
            d = qq - kk + off
            out[8 + h, :, j * 128:(j + 1) * 128] = np.where(np.abs(d) <= 128, -sb_[h] * np.abs(d), NEGB)
    return out


def _slot1(o):
    return 0 if o == 3 else o + 2


def _heads_layer1(qn, kn):
    kt = [(128 * j, 1, 128) for j in range(NE // 128)]
    qt = []
    for m in range(NOWN // 128):
        ty = m if m < 2 else 2
        offs = range(0, 4) if m == 0 else range(-2, 3)
        kl = [(m + o, (ty * 5 + _slot1(o)) * 128) for o in offs if 0 <= m + o < NE // 128]
        qt.append((128 * m, 1, 128, kl, True))
    return [dict(wv=[2048 + 64 * h for h in range(16)], ktiles=kt, pairs=[
        dict(wq=128 * p, wk=(1024 + 128 * p, 1024 + 128 * p + 64), vslot=2 * p, gq=qn, gk=kn, sink=None,
             orow=128 * p, mi=2 * p, qtiles=qt) for p in range(8)])]


def _bias_layer1(rpb, hf):
    out = np.full((16, 128, 15 * 128), NEGB, np.float32)
    loc = np.arange(128)
    for ty in range(3):
        m = ty
        for o in (range(0, 4) if ty == 0 else range(-2, 3)):
            if m + o < 0:
                continue
            uq = 128 * m + loc
            uk = 128 * (m + o) + loc
            tq = uq if hf == 0 else 4095 - uq
            tk = uk if hf == 0 else 4095 - uk
            iq, cq = (tq // 64)[None, :], (tq % 64)[None, :]
            ik, ck = (tk // 64)[:, None], (tk % 64)[:, None]
            rs = np.clip(iq - 4, 0, 56)
            qs = np.clip(cq - 8, 0, 48)
            valid = (ik >= rs) & (ik < rs + 8) & (ck >= qs) & (ck < qs + 16)
            ro = np.clip(ik - iq + 7, 0, 14)
            co = np.clip(ck - cq, -15, 15) + 15
            g = rpb[:, ro, co]
            col = (ty * 5 + _slot1(o)) * 128
            out[:, :, col:col + 128] = np.where(valid[None], g, NEGB)
    return out


def _pk(v):
    return np.ascontiguousarray(np.asarray(v, np.float32).reshape(KC, 128).T)


def build_program(debug=False, upto=99):
    nc = bass.Bass("TRN2", target_bir_lowering=False)

    def din(name, shape, dt=F32):
        return nc.dram_tensor(name, shape, dt, kind="ExternalInput").ap()

    def dint(name, shape, dt):
        return nc.dram_tensor(name, shape, dt, kind="ExternalOutput" if debug else "Internal").ap()

    xT = din('xT', [D, NK])
    g1 = din('g1', [128, KC]); g2 = din('g2', [128, KC]); g3 = din('g3', [128, KC]); g4 = din('g4', [128, KC])
    w_in = din('w_in', [D, 2304]); w_o0 = din('w_o0', [D, D])
    qna = din('qna', [128, 1]); kna = din('kna', [128, 1]); qnb = din('qnb', [128, 1]); knb = din('knb', [128, 1])
    sink = din('sink', [4, 128, 1])
    fg = din('fg', [1, D, FF0]); fu = din('fu', [1, D, FF0]); fd = din('fd', [1, FF0, D])
    w_qkv = din('w_qkv', [D, 3072]); w_o1 = din('w_o1', [D, D])
    qnc = din('qnc', [128, 1]); knc = din('knc', [128, 1])
    m0 = din('m0', [16, 128, 768]); m1 = din('m1', [16, 128, 1920])
    rt = din('rt', [128, KC, NEXP])
    eg = din('eg', [NEXP, D, FF1]); eu = din('eu', [NEXP, D, FF1]); ed = din('ed', [NEXP, FF1, D])
    outT = nc.dram_tensor('outT', [D, NOWN], F32, kind="ExternalOutput").ap()
    h0 = dint('h0', [D, NK], BF16); x1 = dint('x1', [D, NE], F32); o0 = dint('o0', [D, NE], BF16)
    h1 = dint('h1', [D, NE], BF16); o1 = dint('o1', [D, NOWN], BF16)
    with ExitStack() as stack:
        S = Sched(nc, stack)
        if upto >= 1:
            norm_phase(nc, S, xT, h0, g1, NK)
        if upto >= 2:
            attn_phase(nc, S, h0, NK, NE, w_in, _heads_layer0(qna, kna, qnb, knb, sink), o0, m0, 768)
        if upto >= 4:
            ffn_phase(nc, S, xT, x1, g2, fg, fu, fd, FF0, [(0, 1152), (1152, 1152)], pmax=1152, tg=384,
                      oT=o0, wout=w_o0, hout=h1, gain2=g3)
        if upto >= 6:
            attn_phase(nc, S, h1, NE, NOWN, w_qkv, _heads_layer1(qnc, knc), o1, m1, 1920)
        if upto >= 8:
            ffn_phase(nc, S, x1, outT, g4, eg, eu, ed, FF1, [(0, 1024), (1024, 1024)], router=rt,
                      oT=o1, wout=w_o1)
    return nc


def kernel(x, ev_norm1, ev_w_in, ev_qn_a, ev_kn_a, ev_qn_b, ev_kn_b, ev_sink_b, ev_w_out,
           ev_norm2, ev_ffn_gate, ev_ffn_up, ev_ffn_down,
           od_norm1, od_w_qkv, od_qn, od_kn, od_rpb, od_w_out, od_norm2, od_router,
           od_exp_gate, od_exp_up, od_exp_down):
    f = lambda a: np.ascontiguousarray(np.asarray(a, np.float32))
    x = f(x)
    col = lambda v: f(np.tile(np.asarray(v).reshape(-1), 2).reshape(-1, 1))
    shared = dict(
        g1=_pk(ev_norm1[0]), g2=_pk(ev_norm2[0]), g3=_pk(od_norm1[0]), g4=_pk(od_norm2[0]),
        w_in=f(ev_w_in[0]), w_o0=f(ev_w_out[0]), qna=col(ev_qn_a[0]), kna=col(ev_kn_a[0]), qnb=col(ev_qn_b[0]),
        knb=col(ev_kn_b[0]), sink=f(np.repeat(np.asarray(ev_sink_b[0]).reshape(4, 2), 64, axis=1).reshape(4, 128, 1)), fg=f(ev_ffn_gate), fu=f(ev_ffn_up), fd=f(ev_ffn_down),
        w_qkv=f(od_w_qkv[0]), w_o1=f(od_w_out[0]), qnc=col(od_qn[0]), knc=col(od_kn[0]), m0=_masks_layer0(),
        rt=f(np.asarray(od_router[0]).reshape(KC, 128, NEXP).transpose(1, 0, 2)),
        eg=f(od_exp_gate[0]), eu=f(od_exp_up[0]), ed=f(od_exp_down[0]))
    rpb = np.asarray(od_rpb[0], np.float32)
    bias1 = [_bias_layer1(rpb, 0), _bias_layer1(rpb, 1)]
    in_maps = []
    for c in range(8):
        b, hf = c // 2, c % 2
        xs = x[b] if hf == 0 else x[b, ::-1]
        m = dict(shared)
        m['xT'] = np.ascontiguousarray(xs[:NK].T)
        m['m1'] = bias1[hf]
        in_maps.append(m)
    if _DEBUG_HOOK:
        return _DEBUG_HOOK[0](in_maps)
    nc = build_program()
    res = run_bass_kernel_spmd(nc, in_maps, core_ids=list(range(8)))
    out = np.empty((4, 4096, D), np.float32)
    for c in range(8):
        b, hf = c // 2, c % 2
        y = res.results[c]['outT'].T
        if hf == 0:
            out[b, :NOWN] = y
        else:
            out[b, NOWN:] = y[::-1]
    return out
```

```python
import numpy as np
import ml_dtypes
from contextlib import ExitStack
import concourse.bass as bass
import concourse.mybir as mybir
from concourse.bass_utils import run_bass_kernel_spmd

F32 = mybir.dt.float32
BF16 = mybir.dt.bfloat16
ALU = mybir.AluOpType
AF = mybir.ActivationFunctionType

D = 1024
KC = 8
NOWN = 2048
NE = 2304
NK = 3328
FF0 = 2816
FF1 = 3584
NEXP = 8
EPS = 1e-6
DBG = {}


_UN = [0]


def _un(name):
    _UN[0] += 1
    return '%s_u%d' % (name, _UN[0])


class Sem:
    _n = [0]

    def __init__(self, h):
        self.h = h
        self.v = 0
        Sem._n[0] += 1
        self.uid = Sem._n[0]


class Sched:
    ENG = ('pe', 'act', 'dve', 'pool', 'sp')

    def __init__(self, nc, stack):
        self.nc = nc
        self.stack = stack
        self.n = 0
        self.sem = {e: self.new_sem('eng_' + e) for e in self.ENG}
        self.waited = {e: {} for e in self.ENG}
        self.ops = {e: [] for e in self.ENG}

    def new_sem(self, name):
        self.n += 1
        return Sem(self.stack.enter_context(self.nc.semaphore('%s_%d' % (name, self.n))))

    def op(self, eng, fn, deps=(), dma_sem=None, seq=False):
        own = self.sem[eng]
        waits = []
        flat = []
        for t in deps:
            if isinstance(t, list):
                flat.extend(t)
            else:
                flat.append(t)
        deps = flat
        if seq and own.v > 0:
            deps.append((own, own.v))
        for t in deps:
            if t is None:
                continue
            s, v = t
            if self.waited[eng].get(s.uid, 0) >= v:
                continue
            self.waited[eng][s.uid] = v
            waits.append((s, v))
        if dma_sem is None:
            own.v += 1
            ticket = (own, own.v)
            inc = (own, 1)
        else:
            dma_sem.v += 16
            ticket = (dma_sem, dma_sem.v)
            inc = (dma_sem, 16)
        self.ops[eng].append((fn, waits, inc))
        return ticket

    def emit(self, final_waits=()):
        nc = self.nc
        ops = self.ops

        def run(e, lst, extra=()):
            for fn, waits, inc in lst:
                for s, v in waits:
                    e.wait_ge(s.h, v)
                ins = fn(e)
                ins.then_inc(inc[0].h, inc[1])
            for s, v in extra:
                e.wait_ge(s.h, v)

        with nc.Block() as block:
            block.tensor(lambda e: run(e, ops['pe']))
            block.scalar(lambda e: run(e, ops['act']))
            block.vector(lambda e: run(e, ops['dve']))
            block.gpsimd(lambda e: run(e, ops['pool']))
            block.sync(lambda e: run(e, ops['sp'], final_waits))
        self.ops = {e: [] for e in self.ENG}


class Banks:
    def __init__(self, tiles):
        self.tiles = tiles
        self.rel = [None] * len(tiles)
        self.i = 0

    def get(self):
        b = self.i
        self.i = (self.i + 1) % len(self.tiles)
        return b, self.tiles[b], self.rel[b]

    def release(self, b, ticket):
        self.rel[b] = ticket


def groups_of(n, g=512):
    out = []
    s = 0
    while s < n:
        out.append((s, min(g, n - s)))
        s += g
    return out


def ffn_phase(nc, S, xin, xout, gain, wg, wu, wd, ff, parts, router=None, out_sem=None, pmax=1024, tg=512,
              oT=None, wout=None, hout=None, gain2=None):
    E = wg.shape[0]
    moe = router is not None
    PMAX = pmax
    with ExitStack() as st:
        def sb(name, shape, dt):
            return st.enter_context(nc.sbuf_tensor(_un(name), shape, dt))

        yacc = sb('yacc', [128, KC, PMAX], F32)
        hT = sb('hT', [128, KC, PMAX], BF16)
        sq = [sb('sq%d' % i, [128, KC, 512], BF16) for i in range(2)]
        rstd = sb('rstd', [128, PMAX], F32)
        gn = sb('gn', [128, KC], F32)
        ones = sb('ones', [128, 128], BF16)
        epsc = sb('epsc', [128, 1], F32)
        wgb = [sb('wgb%d' % i, [128, KC, 512], BF16) for i in range(2)]
        wub = [sb('wub%d' % i, [128, KC, 512], BF16) for i in range(2)]
        wdb = [sb('wdb%d' % i, [128, 4, D], BF16) for i in range(2)]
        actT = [sb('actT%d' % i, [128, 4, PMAX], BF16) for i in range(2)]
        sil = [sb('sil%d' % i, [128, 512], BF16) for i in range(4)]
        if hout is not None:
            gn2 = sb('gn2', [128, KC], F32)
        fuse_o = oT is not None
        if fuse_o:
            wo = sb('wo', [128, KC, D], BF16)
        if moe:
            cbc = sb('cbc', [128, E, PMAX], BF16)
            wr = sb('wr', [128, KC, E], F32)
            wrb = sb('wrb', [128, KC, E], BF16)
            ident = sb('ident', [128, 128], F32)
            onesf = sb('onesf', [128, 128], F32)
            lg = sb('lg', [128, 8, E], F32)
            eq1 = sb('eq1', [128, 8, E], F32)
            eq2 = sb('eq2', [128, 8, E], F32)
            msk = sb('msk', [128, 8, E], F32)
            comb = sb('comb', [128, 8, E], F32)
            m1t = sb('m1t', [128, 8], F32)
            m2t = sb('m2t', [128, 8], F32)
            dmt = sb('dmt', [128, 8], F32)
            ext = sb('ext', [128, 8], F32)
            g1t = sb('g1t', [128, 8], F32)
            g2t = sb('g2t', [128, 8], F32)
            rsT = sb('rsT', [128, 8], F32)
            inv128 = sb('inv128', [128, 1], F32)
            dg = [sb('dg%d' % i, [128, 4, 128], BF16) for i in range(4)]
            onesb = sb('onesb', [128, 128], BF16)
        ps = [st.enter_context(nc.psum_tensor(_un('ps%d' % i), [128, 512], F32)) for i in range(8)]
        banks = Banks(ps)
        sem_x = S.new_sem('ffn_x')
        sem_c = S.new_sem('ffn_c')
        sem_wa = [S.new_sem('ffn_wa') for _ in range(2)]
        sem_wd = [S.new_sem('ffn_wd') for _ in range(2)]
        sem_st = out_sem if out_sem is not None else S.new_sem('ffn_st')

        if fuse_o:
            t_wo = S.op('pool', lambda e: e.dma_start(out=wo[:], in_=wout.rearrange('(k q) d -> q k d', q=128)),
                        dma_sem=S.new_sem('ffn_wo'))
            sem_o = S.new_sem('ffn_o')
        t_g = S.op('sp', lambda e: e.dma_start(out=gn[:], in_=gain), dma_sem=sem_c)
        t_hs = None
        if hout is not None:
            t_g2 = S.op('sp', lambda e: e.dma_start(out=gn2[:], in_=gain2), dma_sem=S.new_sem('ffn_c3'))
            sem_hs = S.new_sem('ffn_hs')
        t_ones = S.op('pool', lambda e: e.memset(ones[:], 1.0 / D))
        t_eps = S.op('pool', lambda e: e.memset(epsc[:], EPS))
        if moe:
            t_wr = S.op('sp', lambda e: e.dma_start(out=wr[:], in_=router),
                        dma_sem=S.new_sem('ffn_c2'))
            t_c0 = S.op('pool', lambda e: e.memset(ident[:], 0.0))
            t_id = S.op('pool', lambda e: e.affine_select(out=ident[:], in_=ident[:], pattern=[[-1, 128]],
                                                          compare_op=ALU.not_equal, fill=1.0, base=0,
                                                          channel_multiplier=1), deps=[t_c0])
            S.op('pool', lambda e: e.memset(onesf[:], 1.0))
            S.op('pool', lambda e: e.memset(onesb[:], 1.0))
            t_of = S.op('pool', lambda e: e.memset(inv128[:], 1.0 / 128))
            t_wrs = S.op('dve', lambda e: e.tensor_copy(wrb[:], wr[:]), deps=[t_wr])

        blocks = []
        for ex in range(E):
            for (f0, fw) in groups_of(ff, 512):
                blocks.append((ex, f0, fw))
        nblk = len(blocks)

        last_store = None
        pe_a_done = {}
        pe_b_done = {}
        gi = 0
        last_part_reads = []
        for (t0, n) in parts:
            tgs = groups_of(n, tg)
            mm_hist = []
            t_ld = S.op('sp', lambda e, t0=t0, n=n: e.dma_start(
                out=yacc[:, :, 0:n], in_=xin[:, t0:t0 + n].rearrange('(k p) t -> p k t', p=128)),
                deps=last_part_reads, dma_sem=sem_x)
            t_res = {}
            if fuse_o:
                t_lo = S.op('sp', lambda e, t0=t0, n=n: e.dma_start(
                    out=hT[:, :, 0:n], in_=oT[:, t0:t0 + n].rearrange('(k p) t -> p k t', p=128)),
                    deps=last_part_reads, dma_sem=sem_o)
                for (g0, gw) in tgs:
                    t_a = None
                    for d in range(KC):
                        b, pt, rel = banks.get()
                        t_m = None
                        for k in range(KC):
                            t_m = S.op('pe', lambda e, k=k, d=d, g0=g0, gw=gw, pt=pt: e.matmul(
                                pt[:, 0:gw], wo[:, k, d * 128:(d + 1) * 128], hT[:, k, g0:g0 + gw],
                                start=(k == 0), stop=(k == KC - 1)), deps=[t_wo, t_lo, rel] + last_part_reads)
                        t_a = S.op('dve', lambda e, d=d, g0=g0, gw=gw, pt=pt: e.tensor_tensor(
                            yacc[:, d, g0:g0 + gw], pt[:, 0:gw], yacc[:, d, g0:g0 + gw], ALU.add),
                            deps=[t_m, t_ld], seq=True)
                        banks.release(b, t_a)
                    t_res[g0] = t_a
            t_h = []
            for gidx, (g0, gw) in enumerate(tgs):
                sqb = sq[gidx % 2]
                t_sq = None
                for k in range(KC):
                    t_sq = S.op('act', lambda e, k=k, g0=g0, gw=gw, sqb=sqb: e.activation(
                        out=sqb[:, k, 0:gw], in_=yacc[:, k, g0:g0 + gw], func=AF.Square),
                        deps=[t_ld, t_res.get(g0)] + last_part_reads + ([mm_hist[gidx - 2]] if gidx >= 2 else []))
                b, pt, rel = banks.get()
                t_mm = None
                for k in range(KC):
                    t_mm = S.op('pe', lambda e, k=k, gw=gw, sqb=sqb, pt=pt: e.matmul(
                        pt[:, 0:gw], ones[:], sqb[:, k, 0:gw], start=(k == 0), stop=(k == KC - 1)),
                        deps=[t_sq, rel, t_ones])
                mm_hist.append(t_mm)
                t_s = S.op('act', lambda e, g0=g0, gw=gw, pt=pt: e.activation(
                    out=rstd[:, g0:g0 + gw], in_=pt[:, 0:gw], func=AF.Ln, bias=epsc[:, 0:1], scale=1.0), deps=[t_mm, t_eps])
                banks.release(b, t_s)
                t_r = S.op('act', lambda e, g0=g0, gw=gw: e.activation(
                    out=rstd[:, g0:g0 + gw], in_=rstd[:, g0:g0 + gw], func=AF.Exp, scale=-0.5), deps=[t_s])
                for k in range(KC):
                    t_hk = S.op('dve', lambda e, k=k, g0=g0, gw=gw: e.scalar_tensor_tensor(
                        out=hT[:, k, g0:g0 + gw], in0=yacc[:, k, g0:g0 + gw], scalar=gn[:, k:k + 1],
                        in1=rstd[:, g0:g0 + gw], op0=ALU.mult, op1=ALU.mult),
                        deps=[t_g, t_ld, t_r, t_res.get(g0)] + last_part_reads)
                t_h.append(t_hk)
            t_hall = t_h[-1]
            t_cb = None
            if moe:
                ntile = n // 128
                t_l = None
                for ti in range(ntile):
                    c0 = ti * 128
                    b, pt, rel = banks.get()
                    t_mm = None
                    for k in range(KC):
                        t_mm = S.op('pe', lambda e, k=k, c0=c0, pt=pt: e.matmul(
                            pt[:, 0:E], hT[:, k, c0:c0 + 128], wrb[:, k, :], start=(k == 0), stop=(k == KC - 1)),
                            deps=[t_hall, t_wrs, rel])
                    t_l = S.op('dve', lambda e, ti=ti, pt=pt: e.tensor_copy(lg[:, ti, :], pt[:, 0:E]),
                               deps=[t_mm], seq=True)
                    banks.release(b, t_l)
                nt = ntile
                X = mybir.AxisListType.X
                S.op('dve', lambda e, nt=nt: e.tensor_reduce(out=m1t[:, 0:nt], in_=lg[:, 0:nt, :], axis=X, op=ALU.max), seq=True)
                S.op('dve', lambda e, nt=nt: e.tensor_tensor(eq1[:, 0:nt, :], lg[:, 0:nt, :],
                                                      m1t[:, 0:nt].unsqueeze(2).broadcast_to([128, nt, E]), ALU.is_equal), seq=True)
                S.op('dve', lambda e, nt=nt: e.scalar_tensor_tensor(out=msk[:, 0:nt, :], in0=eq1[:, 0:nt, :], scalar=-1e30,
                                                             in1=lg[:, 0:nt, :], op0=ALU.mult, op1=ALU.add), seq=True)
                S.op('dve', lambda e, nt=nt: e.tensor_reduce(out=m2t[:, 0:nt], in_=msk[:, 0:nt, :], axis=X, op=ALU.max), seq=True)
                S.op('dve', lambda e, nt=nt: e.tensor_tensor(eq2[:, 0:nt, :], msk[:, 0:nt, :],
                                                      m2t[:, 0:nt].unsqueeze(2).broadcast_to([128, nt, E]), ALU.is_equal), seq=True)
                t_dm = S.op('dve', lambda e, nt=nt: e.tensor_tensor(dmt[:, 0:nt], m2t[:, 0:nt], m1t[:, 0:nt], ALU.subtract), seq=True)
                t_ex = S.op('act', lambda e, nt=nt: e.activation(out=ext[:, 0:nt], in_=dmt[:, 0:nt], func=AF.Exp), deps=[t_dm])
                S.op('dve', lambda e, nt=nt: e.tensor_scalar(g1t[:, 0:nt], ext[:, 0:nt], 1.0, None, ALU.add), deps=[t_ex], seq=True)
                S.op('dve', lambda e, nt=nt: e.reciprocal(g1t[:, 0:nt], g1t[:, 0:nt]), seq=True)
                S.op('dve', lambda e, nt=nt: e.tensor_tensor(g2t[:, 0:nt], ext[:, 0:nt], g1t[:, 0:nt], ALU.mult), seq=True)
                S.op('dve', lambda e, nt=nt: e.tensor_tensor(eq1[:, 0:nt, :], eq1[:, 0:nt, :],
                                                      g1t[:, 0:nt].unsqueeze(2).broadcast_to([128, nt, E]), ALU.mult), seq=True)
                S.op('dve', lambda e, nt=nt: e.tensor_tensor(eq2[:, 0:nt, :], eq2[:, 0:nt, :],
                                                      g2t[:, 0:nt].unsqueeze(2).broadcast_to([128, nt, E]), ALU.mult), seq=True)
                S.op('dve', lambda e, nt=nt: e.tensor_tensor(comb[:, 0:nt, :], eq1[:, 0:nt, :], eq2[:, 0:nt, :], ALU.add), seq=True)
                if DBG:
                    t_cm = S.op('dve', lambda e, nt=nt: e.tensor_copy(comb[:, 0:nt, :], comb[:, 0:nt, :]), seq=True)
                    for nm, tt in (('ident', ident), ('lg', lg), ('comb', comb), ('eq1', eq1), ('m1t', m1t), ('g1t', g1t)):
                        if nm in DBG:
                            S.op('sp', lambda e, nm=nm, tt=tt: e.dma_start(out=DBG[nm], in_=tt[:]), deps=[t_cm, t_id],
                                 dma_sem=S.new_sem('dbg'))
                dg_rel = [None] * 4
                di = 0
                for ti in range(ntile):
                    c0 = ti * 128
                    for hf in range(E // 4):
                        dgb = dg[di % 4]
                        t_d = S.op('dve', lambda e, ti=ti, hf=hf, dgb=dgb: e.tensor_tensor(
                            dgb[:, :, :], ident[:, :].unsqueeze(1).broadcast_to([128, 4, 128]),
                            comb[:, ti, 4 * hf:4 * hf + 4].unsqueeze(2).broadcast_to([128, 4, 128]), ALU.mult),
                            deps=[dg_rel[di % 4], t_id])
                        b, pt, rel = banks.get()
                        t_bm = S.op('pe', lambda e, dgb=dgb, pt=pt: e.matmul(
                            pt[:, 0:512], onesb[:, :], dgb[:, :, :].rearrange('p e n -> p (e n)'),
                            start=True, stop=True), deps=[t_d, rel, t_of])
                        dg_rel[di % 4] = t_bm
                        di += 1
                        t_cb = S.op('act', lambda e, hf=hf, c0=c0, pt=pt: e.activation(
                            out=cbc[:, 4 * hf:4 * hf + 4, c0:c0 + 128],
                            in_=pt[:, 0:512].rearrange('p (e n) -> p e n', e=4), func=AF.Copy),
                            deps=[t_bm] + last_part_reads)
                        banks.release(b, t_cb)
            sil_rel = [None] * 4
            nb = nblk
            a_tickets = {}

            def pass_a(i):
                ex, f0, fw = blocks[i]
                g = gi + i
                par = g % 2
                dep_free = [pe_a_done.get(g - 2)]
                S.op('pool', lambda e: e.dma_start(
                    out=wgb[par][:, :, 0:fw], in_=wg[ex, :, f0:f0 + fw].rearrange('(k p) f -> p k f', p=128)),
                    deps=dep_free, dma_sem=sem_wa[par])
                t_w = S.op('pool', lambda e: e.dma_start(
                    out=wub[par][:, :, 0:fw], in_=wu[ex, :, f0:f0 + fw].rearrange('(k p) f -> p k f', p=128)),
                    deps=dep_free, dma_sem=sem_wa[par])
                t_wdl = S.op('pool', lambda e: e.dma_start(
                    out=wdb[par][:, 0:fw // 128, :], in_=wd[ex, f0:f0 + fw, :].rearrange('(c p) d -> p c d', p=128)),
                    deps=[pe_b_done.get(g - 2)], dma_sem=sem_wd[par])
                a_tickets[i] = t_wdl
                t_last = None
                t_act_last = None
                for c in range(fw // 128):
                    for (g0, gw) in tgs:
                        bg, pg, relg = banks.get()
                        for k in range(KC):
                            t_g1 = S.op('pe', lambda e, k=k, c=c, g0=g0, gw=gw, pg=pg: e.matmul(
                                pg[:, 0:gw], wgb[par][:, k, c * 128:(c + 1) * 128], hT[:, k, g0:g0 + gw],
                                start=(k == 0), stop=(k == KC - 1)), deps=[t_w, t_hall, relg])
                        bu, pu, relu = banks.get()
                        for k in range(KC):
                            t_u1 = S.op('pe', lambda e, k=k, c=c, g0=g0, gw=gw, pu=pu: e.matmul(
                                pu[:, 0:gw], wub[par][:, k, c * 128:(c + 1) * 128], hT[:, k, g0:g0 + gw],
                                start=(k == 0), stop=(k == KC - 1)), deps=[t_w, t_hall, relu])
                        t_last = t_u1
                        si = sil_rel.index(min(sil_rel, key=lambda t: -1 if t is None else t[1]))
                        sbuf_s = sil[si]
                        t_si = S.op('act', lambda e, gw=gw, pg=pg, sbuf_s=sbuf_s: e.activation(
                            out=sbuf_s[:, 0:gw], in_=pg[:, 0:gw], func=AF.Silu), deps=[t_g1, sil_rel[si]])
                        banks.release(bg, t_si)
                        if moe:
                            t_si = S.op('dve', lambda e, g0=g0, gw=gw, ex=ex, sbuf_s=sbuf_s: e.tensor_tensor(
                                sbuf_s[:, 0:gw], sbuf_s[:, 0:gw], cbc[:, ex, g0:g0 + gw], ALU.mult),
                                deps=[t_si, t_cb])
                        t_m = S.op('dve', lambda e, c=c, g0=g0, gw=gw, pu=pu, sbuf_s=sbuf_s: e.tensor_tensor(
                            actT[par][:, c, g0:g0 + gw], pu[:, 0:gw], sbuf_s[:, 0:gw], ALU.mult),
                            deps=[t_u1, t_si, pe_b_done.get(g - 2)])
                        sil_rel[si] = t_m
                        banks.release(bu, t_m)
                        t_act_last = t_m
                pe_a_done[g] = t_last
                return t_act_last

            def pass_b(i, t_act):
                ex, f0, fw = blocks[i]
                g = gi + i
                par = g % 2
                nc_ = fw // 128
                t_last = None
                t_acc = None
                for d in range(KC):
                    for (g0, gw) in tgs:
                        by, py, rely = banks.get()
                        for c in range(nc_):
                            t_y = S.op('pe', lambda e, c=c, d=d, g0=g0, gw=gw, py=py: e.matmul(
                                py[:, 0:gw], wdb[par][:, c, d * 128:(d + 1) * 128], actT[par][:, c, g0:g0 + gw],
                                start=(c == 0), stop=(c == nc_ - 1)), deps=[a_tickets[i], t_act, rely])
                        t_last = t_y
                        t_acc = S.op('dve', lambda e, d=d, g0=g0, gw=gw, py=py: e.tensor_tensor(
                            yacc[:, d, g0:g0 + gw], py[:, 0:gw], yacc[:, d, g0:g0 + gw], ALU.add), deps=[t_y])
                        banks.release(by, t_acc)
                pe_b_done[g] = t_last
                return t_acc

            t_acts = {}
            t_acc_last = None
            t_acts[0] = pass_a(0)
            for i in range(nb):
                if i + 1 < nb:
                    t_acts[i + 1] = pass_a(i + 1)
                t_acc_last = pass_b(i, t_acts[i])
            gi += nb
            last_store = S.op('sp', lambda e, t0=t0, n=n: e.dma_start(
                out=xout[:, t0:t0 + n].rearrange('(k p) t -> p k t', p=128), in_=yacc[:, :, 0:n]),
                deps=[t_acc_last], dma_sem=sem_st)
            last_part_reads = [last_store, pe_a_done[gi - 1], pe_b_done[gi - 1]]
            if hout is not None:
                t_hk = None
                for gidx, (g0, gw) in enumerate(tgs):
                    sqb = sq[gidx % 2]
                    t_sq = None
                    for k in range(KC):
                        t_sq = S.op('act', lambda e, k=k, g0=g0, gw=gw, sqb=sqb: e.activation(
                            out=sqb[:, k, 0:gw], in_=yacc[:, k, g0:g0 + gw], func=AF.Square),
                            deps=[t_acc_last] + mm_hist[-2:])
                    b, pt, rel = banks.get()
                    t_mm = None
                    for k in range(KC):
                        t_mm = S.op('pe', lambda e, k=k, gw=gw, sqb=sqb, pt=pt: e.matmul(
                            pt[:, 0:gw], ones[:], sqb[:, k, 0:gw], start=(k == 0), stop=(k == KC - 1)),
                            deps=[t_sq, rel, t_ones])
                    mm_hist.append(t_mm)
                    t_s = S.op('act', lambda e, g0=g0, gw=gw, pt=pt: e.activation(
                        out=rstd[:, g0:g0 + gw], in_=pt[:, 0:gw], func=AF.Ln, bias=epsc[:, 0:1], scale=1.0),
                        deps=[t_mm, t_eps, t_hall])
                    banks.release(b, t_s)
                    t_r = S.op('act', lambda e, g0=g0, gw=gw: e.activation(
                        out=rstd[:, g0:g0 + gw], in_=rstd[:, g0:g0 + gw], func=AF.Exp, scale=-0.5), deps=[t_s])
                    for k in range(KC):
                        t_hk = S.op('dve', lambda e, k=k, g0=g0, gw=gw: e.scalar_tensor_tensor(
                            out=hT[:, k, g0:g0 + gw], in0=yacc[:, k, g0:g0 + gw], scalar=gn2[:, k:k + 1],
                            in1=rstd[:, g0:g0 + gw], op0=ALU.mult, op1=ALU.mult),
                            deps=[t_g2, t_r, t_acc_last, pe_a_done[gi - 1]])
                t_hs = S.op('sp', lambda e, t0=t0, n=n: e.dma_start(
                    out=hout[:, t0:t0 + n].rearrange('(k p) t -> p k t', p=128), in_=hT[:, :, 0:n]),
                    deps=[t_hk], dma_sem=sem_hs)
                last_part_reads = last_part_reads + [t_hs, t_hk]
        S.emit(final_waits=[last_store] + ([t_hs] if t_hs is not None else []))
    return last_store


def norm_phase(nc, S, xin, hout, gain, n):
    with ExitStack() as st:
        def sb(name, shape, dt):
            return st.enter_context(nc.sbuf_tensor(_un(name), shape, dt))
        NB = 4
        xb = [sb('nx%d' % i, [128, KC, 512], F32) for i in range(NB)]
        hb = [sb('nh%d' % i, [128, KC, 512], BF16) for i in range(NB)]
        sq = [sb('nsq%d' % i, [128, KC, 512], BF16) for i in range(NB)]
        rs = [sb('nrs%d' % i, [128, 512], F32) for i in range(NB)]
        gn = sb('ngn', [128, KC], F32)
        ones = sb('nones', [128, 128], BF16)
        epsc = sb('nepsc', [128, 1], F32)
        ps = [st.enter_context(nc.psum_tensor(_un('nps%d' % i), [128, 512], F32)) for i in range(NB)]
        t_g = S.op('sp', lambda e: e.dma_start(out=gn[:], in_=gain), dma_sem=S.new_sem('n_g'))
        S.op('pool', lambda e: e.memset(ones[:], 1.0 / D))
        t_o = S.op('pool', lambda e: e.memset(epsc[:], EPS))
        sx = [S.new_sem('n_x') for _ in range(NB)]
        so = [S.new_sem('n_o') for _ in range(NB)]
        prev = [None] * NB
        last = None
        for gi, (g0, gw) in enumerate(groups_of(n, 512)):
            p = gi % NB
            t_ld = S.op('sp', lambda e, g0=g0, gw=gw, p=p: e.dma_start(
                out=xb[p][:, :, 0:gw], in_=xin[:, g0:g0 + gw].rearrange('(k q) t -> q k t', q=128)),
                deps=[prev[p]], dma_sem=sx[p])
            t_sq = None
            for k in range(KC):
                t_sq = S.op('act', lambda e, k=k, gw=gw, p=p: e.activation(
                    out=sq[p][:, k, 0:gw], in_=xb[p][:, k, 0:gw], func=AF.Square), deps=[t_ld, prev[p]])
            for k in range(KC):
                t_mm = S.op('pe', lambda e, k=k, gw=gw, p=p: e.matmul(
                    ps[p][:, 0:gw], ones[:], sq[p][:, k, 0:gw], start=(k == 0), stop=(k == KC - 1)),
                    deps=[t_sq, t_o, prev[p]])
            t_s = S.op('act', lambda e, gw=gw, p=p: e.activation(
                out=rs[p][:, 0:gw], in_=ps[p][:, 0:gw], func=AF.Ln, bias=epsc[:, 0:1], scale=1.0), deps=[t_mm, t_o])
            t_r = S.op('act', lambda e, gw=gw, p=p: e.activation(
                out=rs[p][:, 0:gw], in_=rs[p][:, 0:gw], func=AF.Exp, scale=-0.5), deps=[t_s])
            for k in range(KC):
                t_h = S.op('dve', lambda e, k=k, gw=gw, p=p: e.scalar_tensor_tensor(
                    out=hb[p][:, k, 0:gw], in0=xb[p][:, k, 0:gw], scalar=gn[:, k:k + 1], in1=rs[p][:, 0:gw],
                    op0=ALU.mult, op1=ALU.mult), deps=[t_g, t_r, prev[p]])
            last = S.op('pool', lambda e, g0=g0, gw=gw, p=p: e.dma_start(
                out=hout[:, g0:g0 + gw].rearrange('(k q) t -> q k t', q=128), in_=hb[p][:, :, 0:gw]),
                deps=[t_h], dma_sem=so[p])
            prev[p] = last
        S.emit(final_waits=[t for t in prev if t is not None])


def attn_phase(nc, S, hsrc, nk, nq, w, groups, oT, masks_dram, M):
    with ExitStack() as st:
        def sb(name, shape, dt):
            return st.enter_context(nc.sbuf_tensor(_un(name), shape, dt))
        maxkt = max(len(g['ktiles']) for g in groups)
        maxsl = max(len(g['wv']) for g in groups)
        hTs = sb('a_h', [128, KC, nk], BF16)
        va = sb('a_va', [128, maxkt, maxsl * 64], BF16)
        wva = sb('a_wv', [128, KC, maxsl * 64], BF16)
        wq = [sb('a_wq%d' % i, [128, KC, 128], BF16) for i in range(2)]
        wk = [sb('a_wk%d' % i, [128, KC, 128], BF16) for i in range(2)]
        qAB = [sb('a_qAB%d' % i, [128, 2, nq], BF16) for i in range(2)]
        kT = [sb('a_kT%d' % i, [128, nk], BF16) for i in range(2)]
        onum = sb('a_on', [128, nq], F32)
        oden = sb('a_od', [128, nq], F32)
        osb = sb('a_osb', [128, nq], BF16)
        mb = sb('a_mb', [128, 2, M], F32)
        mk = [sb('a_mk%d' % i, [128, 2, M], BF16) for i in range(2)]
        sqb = [sb('a_sq%d' % i, [128, 256], BF16) for i in range(3)]
        rsb = [sb('a_rs%d' % i, [128, 256], F32) for i in range(3)]
        gq = [sb('a_gq%d' % i, [128, 1], F32) for i in range(2)]
        gk = [sb('a_gk%d' % i, [128, 1], F32) for i in range(2)]
        esk = [sb('a_esk%d' % i, [128, 1], F32) for i in range(2)]
        bd = sb('a_bd', [128, 128], BF16)
        onesb = sb('a_ones', [128, 128], BF16)
        epsc = sb('a_epsc', [128, 1], F32)
        pbuf = [sb('a_p%d' % i, [128, 512], BF16) for i in range(6)]
        psS = [st.enter_context(nc.psum_tensor(_un('a_ps%d' % i), [128, 512], F32)) for i in range(2)]
        psO = [st.enter_context(nc.psum_tensor(_un('a_po%d' % i), [128, 512], F32)) for i in range(2)]
        psD = [st.enter_context(nc.psum_tensor(_un('a_pd%d' % i), [128, 512], F32)) for i in range(2)]
        psP = [st.enter_context(nc.psum_tensor(_un('a_pp%d' % i), [128, 512], F32)) for i in range(2)]
        t_h = S.op('sp', lambda e: e.dma_start(
            out=hTs[:], in_=hsrc[:, 0:nk].rearrange('(k q) t -> q k t', q=128)), dma_sem=S.new_sem('a_h'))
        S.op('pool', lambda e: e.memset(bd[:], 0.0))
        S.op('pool', lambda e: e.memset(bd[0:64, 0:64], 1.0 / 64), seq=True)
        S.op('pool', lambda e: e.memset(bd[64:128, 64:128], 1.0 / 64), seq=True)
        S.op('pool', lambda e: e.memset(onesb[:], 1.0))
        S.op('pool', lambda e: e.memset(epsc[:], EPS))
        for i in range(2):
            S.op('pool', lambda e, i=i: e.memset(qAB[i][64:128, 0, :], 0.0))
            S.op('pool', lambda e, i=i: e.memset(qAB[i][0:64, 1, :], 0.0))
        t_c = S.op('pool', lambda e: e.memset(epsc[:], EPS), seq=True)
        sem_w = [S.new_sem('a_w') for _ in range(2)]
        sem_g = [S.new_sem('a_g') for _ in range(2)]
        sem_mb = S.new_sem('a_mb')
        sem_sk = [S.new_sem('a_sk') for _ in range(2)]
        sem_wv = S.new_sem('a_wv')
        sem_o = S.new_sem('a_o')
        brel = [None] * 4
        psALL = psS + psP
        pn = {'i': 0, 'sq': 0, 'sqrel': [None] * 3}
        ring = {'S': 0, 'O': 0, 'Orel': [None] * 2, 'P': 0, 'Prel': [None] * 6}
        fin_hist = []
        t_store = None
        t_osb_free = None
        t_acc_free = None
        t_mb_free = None
        gdone = None
        pi_glob = 0
        prev_tq = None

        PENDING = 'PENDING'
        NIDLE = 12
        G = {'t_mb_free': None, 'prev_tq': None, 't_wv': None}
        fin_by_idx = {}

        def proj_pair(wt, gt, n, t_w, t_gl, dsts, extra, out):
            last = None
            for (g0, gw) in groups_of(n, 256):
                r = pn['i'] % 2
                pn['i'] += 1
                q = pn['sq'] % 3
                pn['sq'] += 1
                pp = psP[r]
                rel = brel[2 + r]
                srel = pn['sqrel'][q]
                for k in range(KC):
                    t1 = S.op('pe', lambda e, k=k, g0=g0, gw=gw, pp=pp: e.matmul(
                        pp[:, 0:gw], wt[:, k, :], hTs[:, k, g0:g0 + gw], start=(k == 0), stop=(k == KC - 1)),
                        deps=[t_w, t_h, rel] + extra)
                t2 = S.op('act', lambda e, gw=gw, pp=pp, q=q: e.activation(
                    out=sqb[q][:, 0:gw], in_=pp[:, 0:gw], func=AF.Square), deps=[t1, srel])
                brel[2 + r] = PENDING
                yield
                t3 = S.op('pe', lambda e, gw=gw, pp=pp, q=q: e.matmul(
                    pp[:, 256:256 + gw], bd[:], sqb[q][:, 0:gw], start=True, stop=True), deps=[t2, t_c])
                t4 = S.op('act', lambda e, gw=gw, pp=pp, q=q: e.activation(
                    out=rsb[q][:, 0:gw], in_=pp[:, 256:256 + gw], func=AF.Ln, bias=epsc[:, 0:1], scale=1.0),
                    deps=[t3, srel, t_c])
                t5 = S.op('act', lambda e, gw=gw, q=q: e.activation(
                    out=rsb[q][:, 0:gw], in_=rsb[q][:, 0:gw], func=AF.Exp, scale=-0.5), deps=[t4])
                for (dst, r0, r1) in dsts:
                    last = S.op('dve', lambda e, g0=g0, gw=gw, pp=pp, q=q, dst=dst, r0=r0, r1=r1:
                                e.scalar_tensor_tensor(out=dst[r0:r1, g0:g0 + gw], in0=pp[r0:r1, 0:gw],
                                                       scalar=gt[r0:r1, 0:1], in1=rsb[q][r0:r1, 0:gw],
                                                       op0=ALU.mult, op1=ALU.mult), deps=[t5, t_gl, t1] + extra)
                brel[2 + r] = last
                pn['sqrel'][q] = last
                yield
            out.append(last)

        def prep(pr, pidx, stt):
            b2 = pidx % 2
            free2 = fin_by_idx.get(pidx - 2)
            thr = [free2, G['t_wv'], G['prev_tq']]
            S.op('pool', lambda e, pr=pr, b2=b2: e.dma_start(
                out=wq[b2][:], in_=w[:, pr['wq']:pr['wq'] + 128].rearrange('(k q) c -> q k c', q=128)),
                deps=thr, dma_sem=sem_w[b2])
            for hh in range(2):
                S.op('pool', lambda e, pr=pr, b2=b2, hh=hh: e.dma_start(
                    out=wk[b2][:, :, hh * 64:(hh + 1) * 64],
                    in_=w[:, pr['wk'][hh]:pr['wk'][hh] + 64].rearrange('(k q) c -> q k c', q=128)),
                    deps=thr, dma_sem=sem_w[b2])
            t_w = (sem_w[b2], sem_w[b2].v)
            S.op('sp', lambda e, pr=pr, b2=b2: e.dma_start(out=gq[b2][:], in_=pr['gq']), deps=[free2],
                 dma_sem=sem_g[b2])
            S.op('sp', lambda e, pr=pr, b2=b2: e.dma_start(out=gk[b2][:], in_=pr['gk']), deps=[free2],
                 dma_sem=sem_g[b2])
            t_gl = (sem_g[b2], sem_g[b2].v)
            t_ml = S.op('sp', lambda e, pr=pr: e.dma_start(
                out=mb[:], in_=masks_dram[pr['mi']:pr['mi'] + 2].rearrange('h p m -> p h m')),
                deps=[G['t_mb_free']], dma_sem=sem_mb)
            t_sk = None
            if pr.get('sink') is not None:
                t_sk = S.op('sp', lambda e, pr=pr, b2=b2: e.dma_start(out=esk[b2][:], in_=pr['sink']),
                            deps=[free2], dma_sem=sem_sk[b2])
            for _ in range(NIDLE + 1):
                yield
            oq, ok = [], []
            yield from proj_pair(wq[b2], gq[b2], nq, t_w, t_gl,
                                 [(qAB[b2][:, 0, :], 0, 64), (qAB[b2][:, 1, :], 64, 128)], [free2], oq)
            yield from proj_pair(wk[b2], gk[b2], nk, t_w, t_gl, [(kT[b2], 0, 128)], [free2], ok)
            t_mk = S.op('act', lambda e, b2=b2: e.activation(out=mk[b2][:], in_=mb[:], func=AF.Exp),
                        deps=[t_ml, free2])
            G['t_mb_free'] = t_mk
            if t_sk is not None:
                t_sk = S.op('act', lambda e, b2=b2: e.activation(out=esk[b2][:], in_=esk[b2][:], func=AF.Exp),
                            deps=[t_sk])
            G['prev_tq'] = oq[0]
            stt.update(b2=b2, t_q=oq[0], t_k=ok[0], t_mk=t_mk, t_sk=t_sk)

        flat = [(grp, pr) for grp in groups for pr in grp['pairs']]
        nsteps = 1 + NIDLE + 2 * (len(groups_of(nq, 256)) + len(groups_of(nk, 256)))
        cur_stt = {}
        for _ in prep(flat[0][1], 0, cur_stt):
            pass
        cur_grp = None
        for pidx, (grp, pr) in enumerate(flat):
            if grp is not cur_grp:
                cur_grp = grp
                kts = grp['ktiles']
                nsl = len(grp['wv'])
                t_wv = None
                for si, c0 in enumerate(grp['wv']):
                    t_wv = S.op('pool', lambda e, si=si, c0=c0: e.dma_start(
                        out=wva[:, :, si * 64:(si + 1) * 64], in_=w[:, c0:c0 + 64].rearrange('(k q) c -> q k c', q=128)),
                        deps=[gdone], dma_sem=sem_wv)
                G['t_wv'] = t_wv
                t_v = None
                for ti, (s0, stp, cnt) in enumerate(kts):
                    for c0 in range(0, nsl * 64, 512):
                        cw = min(512, nsl * 64 - c0)
                        r = pn['i'] % 2
                        pn['i'] += 1
                        pp = psP[r]
                        rel = brel[2 + r]
                        assert rel is not PENDING
                        for k in range(KC):
                            t1 = S.op('pe', lambda e, k=k, s0=s0, stp=stp, cnt=cnt, pp=pp, c0=c0, cw=cw: e.matmul(
                                pp[0:cnt, 0:cw], hTs[:, k, s0:s0 + stp * (cnt - 1) + 1:stp], wva[:, k, c0:c0 + cw],
                                start=(k == 0), stop=(k == KC - 1)), deps=[t_wv, t_h, rel, gdone])
                        t_v = S.op('act', lambda e, ti=ti, cnt=cnt, pp=pp, c0=c0, cw=cw: e.activation(
                            out=va[0:cnt, ti, c0:c0 + cw], in_=pp[0:cnt, 0:cw], func=AF.Copy), deps=[t1, gdone])
                        brel[2 + r] = t_v
            if True:
                stt = cur_stt
                b2, t_q, t_k, t_mk, t_sk = stt['b2'], stt['t_q'], stt['t_k'], stt['t_mk'], stt['t_sk']
                nxt, nxt_stt = None, {}
                if pidx + 1 < len(flat):
                    nxt = prep(flat[pidx + 1][1], pidx + 1, nxt_stt)
                vs = pr['vslot'] * 64
                seq = []
                for (qs, qstp, qn, klist, first) in pr['qtiles']:
                    qsl = slice(qs, qs + qstp * (qn - 1) + 1, qstp)
                    nkt = len(klist)
                    qt = dict(qsl=qsl, qn=qn, nkt=nkt, first=first)
                    for i in range(0, nkt, 2):
                        seq.append(dict(qt=qt, ch=klist[i:i + 2], j0=i, lastc=(i + 2 >= nkt)))

                def emit_scores(c):
                    qt = c['qt']
                    qsl, qn = qt['qsl'], qt['qn']
                    ch = c['ch']
                    while True:
                        si = ring['S'] % 4
                        ring['S'] += 1
                        if brel[si] is not PENDING:
                            break
                    pS = psALL[si]
                    srel = brel[si]
                    pi_ = ring['P'] % 6
                    ring['P'] += 1
                    pb = pbuf[pi_]
                    prel = ring['Prel'][pi_]
                    nch = len(ch)
                    t_s = None
                    for jj, (kt, moff) in enumerate(ch):
                        ks0, kstp, kcnt = kts[kt]
                        ksl = slice(ks0, ks0 + kstp * (kcnt - 1) + 1, kstp)
                        t_s = S.op('pe', lambda e, jj=jj, ksl=ksl, kcnt=kcnt, qsl=qsl, qn=qn, pS=pS, b2=b2: e.matmul(
                            pS[0:kcnt, :].rearrange('p (h c) -> p h c', h=2)[:, :, jj * 128:jj * 128 + qn],
                            kT[b2][:, ksl], qAB[b2][:, :, qsl], start=True, stop=True), deps=[t_q, t_k, srel])
                    t_e = S.op('act', lambda e, pS=pS, pb=pb, nch=nch: e.activation(
                        out=pb[:, :].rearrange('p (h c) -> p h c', h=2)[:, :, 0:nch * 128],
                        in_=pS[:, :].rearrange('p (h c) -> p h c', h=2)[:, :, 0:nch * 128],
                        func=AF.Exp, scale=0.125), deps=[t_s, prel])
                    brel[si] = t_e
                    contig = (nch == 1 or ch[1][1] == ch[0][1] + 128)
                    t_m = None
                    for hh in range(2):
                        if contig:
                            m0_ = ch[0][1]
                            wd_ = (nch - 1) * 128 + qn
                            t_m = S.op('dve', lambda e, hh=hh, m0_=m0_, wd_=wd_, pb=pb, b2=b2: e.tensor_tensor(
                                pb[:, hh * 256:hh * 256 + wd_], pb[:, hh * 256:hh * 256 + wd_],
                                mk[b2][:, hh, m0_:m0_ + wd_], ALU.mult), deps=[t_e, t_mk])
                        else:
                            for jj, (kt, moff) in enumerate(ch):
                                t_m = S.op('dve', lambda e, hh=hh, jj=jj, moff=moff, pb=pb, b2=b2, qn=qn: e.tensor_tensor(
                                    pb[:, hh * 256 + jj * 128:hh * 256 + jj * 128 + qn],
                                    pb[:, hh * 256 + jj * 128:hh * 256 + jj * 128 + qn],
                                    mk[b2][:, hh, moff:moff + qn], ALU.mult), deps=[t_e, t_mk])
                    c['pb'] = pb
                    c['pi'] = pi_
                    c['t_m'] = t_m

                def emit_pv(c):
                    qt = c['qt']
                    qsl, qn, nkt, first = qt['qsl'], qt['qn'], qt['nkt'], qt['first']
                    if c['j0'] == 0:
                        oi = ring['O'] % 2
                        ring['O'] += 1
                        qt['oi'] = oi
                        qt['orel'] = ring['Orel'][oi]
                    oi = qt['oi']
                    orel = qt['orel']
                    pO = psO[oi]
                    pD = psD[oi]
                    pb = c['pb']
                    t_m = c['t_m']
                    t_pv = None
                    for jj, (kt, moff) in enumerate(c['ch']):
                        kcnt = kts[kt][2]
                        jdone = c['j0'] + jj
                        st_ = (jdone == 0)
                        sp_ = (jdone == nkt - 1)
                        rhs = lambda pb=pb, jj=jj, kcnt=kcnt, qn=qn: pb[0:kcnt, :].rearrange(
                            'p (h c) -> p h c', h=2)[:, :, jj * 128:jj * 128 + qn]
                        S.op('pe', lambda e, kt=kt, kcnt=kcnt, qn=qn, pO=pO, rhs=rhs, st_=st_, sp_=sp_, vs=vs: e.matmul(
                            pO[:, 0:256].rearrange('p (h c) -> p h c', h=2)[:, :, 0:qn],
                            va[0:kcnt, kt, vs:vs + 128], rhs(), start=st_, stop=sp_),
                            deps=[t_m, t_v, orel])
                        t_pv = S.op('pe', lambda e, kcnt=kcnt, qn=qn, pD=pD, rhs=rhs, st_=st_, sp_=sp_: e.matmul(
                            pD[:, 0:256].rearrange('p (h c) -> p h c', h=2)[:, :, 0:qn],
                            onesb[0:kcnt, :], rhs(), start=st_, stop=sp_), deps=[t_m, orel])
                    ring['Prel'][c['pi']] = t_pv
                    if not c['lastc']:
                        return
                    evs = []
                    for (dst, pX) in ((onum, pO), (oden, pD)):
                        for hh in range(2):
                            r0, r1 = hh * 64, hh * 64 + 64
                            src = pX[r0:r1, hh * 128:hh * 128 + qn]
                            osl = dst[r0:r1, qsl]
                            if first:
                                if hh == 0:
                                    tk = S.op('act', lambda e, osl=osl, src=src: e.activation(out=osl, in_=src,
                                                                                              func=AF.Copy),
                                              deps=[t_pv, t_acc_free])
                                else:
                                    tk = S.op('dve', lambda e, osl=osl, src=src: e.tensor_copy(osl, src),
                                              deps=[t_pv, t_acc_free])
                                firsts[tk[0].uid] = tk
                            else:
                                tk = S.op('dve', lambda e, osl=osl, src=src: e.tensor_tensor(osl, src, osl, ALU.add),
                                          deps=[t_pv] + list(firsts.values()))
                                adds.append(tk)
                            evs.append(tk)
                    ring['Orel'][oi] = evs

                firsts = {}
                adds = []
                LA = 3
                per = -(-nsteps // max(1, len(seq) - LA - 2))
                for i_ in range(min(LA, len(seq))):
                    emit_scores(seq[i_])
                for i_, c in enumerate(seq):
                    if i_ + LA < len(seq):
                        emit_scores(seq[i_ + LA])
                    emit_pv(c)
                    if nxt is not None:
                        for _ in range(per):
                            if next(nxt, PENDING) is PENDING:
                                nxt = None
                                break
                if nxt is not None:
                    for _ in nxt:
                        pass
                last_add = adds[-1] if adds else None
                fin = list(firsts.values()) + [last_add]
                if t_sk is not None:
                    t_x = S.op('dve', lambda e, b2=b2: e.tensor_scalar(oden[:, :], oden[:, :], esk[b2][:, 0:1], None,
                                                                       ALU.add), deps=fin + [t_sk], seq=True)
                    fin = fin + [t_x]
                t_rd = S.op('act', lambda e: e.activation(out=oden[:, :], in_=oden[:, :], func=AF.Ln), deps=fin)
                t_rd = S.op('act', lambda e: e.activation(out=oden[:, :], in_=oden[:, :], func=AF.Exp, scale=-1.0),
                            deps=[t_rd])
                t_n = S.op('dve', lambda e: e.tensor_tensor(osb[:, :], onum[:, :], oden[:, :], ALU.mult),
                           deps=[t_rd, t_store] + fin)
                t_acc_free = t_n
                t_store = S.op('sp', lambda e, pr=pr: e.dma_start(out=oT[pr['orow']:pr['orow'] + 128, :], in_=osb[:, :]),
                               deps=[t_n], dma_sem=sem_o)
                fin_by_idx[pidx] = t_n
                gdone = t_n
                cur_stt = nxt_stt
        S.emit(final_waits=[t_store])


def oproj_phase(nc, S, oT, wout, xin, xout, n):
    with ExitStack() as st:
        def sb(name, shape, dt):
            return st.enter_context(nc.sbuf_tensor(_un(name), shape, dt))
        wo = sb('o_w', [128, KC, D], BF16)
        NB = 4
        ob = [sb('o_o%d' % i, [128, KC, 512], BF16) for i in range(NB)]
        xb = [sb('o_x%d' % i, [128, KC, 512], F32) for i in range(NB)]
        ps = [st.enter_context(nc.psum_tensor(_un('o_ps%d' % i), [128, 512], F32)) for i in range(4)]
        banks = Banks(ps)
        t_w = S.op('pool', lambda e: e.dma_start(out=wo[:], in_=wout.rearrange('(k q) d -> q k d', q=128)),
                   dma_sem=S.new_sem('o_w'))
        sl = [S.new_sem('o_l') for _ in range(NB)]
        ss = [S.new_sem('o_s') for _ in range(NB)]
        prev = [None] * NB
        for gi, (g0, gw) in enumerate(groups_of(n, 512)):
            p = gi % NB
            S.op('sp', lambda e, g0=g0, gw=gw, p=p: e.dma_start(
                out=ob[p][:, :, 0:gw], in_=oT[:, g0:g0 + gw].rearrange('(k q) t -> q k t', q=128)),
                deps=[prev[p]], dma_sem=sl[p])
            t_l = S.op('sp', lambda e, g0=g0, gw=gw, p=p: e.dma_start(
                out=xb[p][:, :, 0:gw], in_=xin[:, g0:g0 + gw].rearrange('(k q) t -> q k t', q=128)),
                deps=[prev[p]], dma_sem=sl[p])
            t_a = None
            for d in range(KC):
                b, pt, rel = banks.get()
                for k in range(KC):
                    t_m = S.op('pe', lambda e, k=k, d=d, gw=gw, p=p, pt=pt: e.matmul(
                        pt[:, 0:gw], wo[:, k, d * 128:(d + 1) * 128], ob[p][:, k, 0:gw],
                        start=(k == 0), stop=(k == KC - 1)), deps=[t_w, t_l, rel])
                t_a = S.op('dve', lambda e, d=d, gw=gw, p=p, pt=pt: e.tensor_tensor(
                    xb[p][:, d, 0:gw], pt[:, 0:gw], xb[p][:, d, 0:gw], ALU.add), deps=[t_m, t_l], seq=True)
                banks.release(b, t_a)
            prev[p] = S.op('act', lambda e, g0=g0, gw=gw, p=p: e.dma_start(
                out=xout[:, g0:g0 + gw].rearrange('(k q) t -> q k t', q=128), in_=xb[p][:, :, 0:gw]),
                deps=[t_a], dma_sem=ss[p])
        S.emit(final_waits=[t for t in prev if t is not None])


NEGB = -30000.0
_DEBUG_HOOK = []
DIL = (1, 4, 16)


def _alibi():
    s = np.exp2(-8.0 * np.arange(1, 17) / 16).astype(np.float32)
    return s[0::2], s[1::2]


def _heads_layer0(qn_a, kn_a, qn_b, kn_b, sinkp):
    ktiles = []
    tid = {}
    for ci, r in enumerate(DIL):
        lk = NE // r + 64
        for p in range(r):
            for j, (l0, cnt) in enumerate(groups_of(lk, 128)):
                tid[(ci, p, j)] = len(ktiles)
                ktiles.append((l0 * r + p, r, cnt))
    qtiles = []
    for ci, r in enumerate(DIL):
        lq = NE // r
        lo, hi = ci * 256, ci * 256 + 128
        for p in range(r):
            qtiles.append((p, r, 64, [(tid[(ci, p, 0)], hi + 64)], ci == 0))
            m = 0
            l0 = 64
            while l0 < lq:
                qn = min(128, lq - l0)
                qtiles.append((l0 * r + p, r, qn, [(tid[(ci, p, m)], lo), (tid[(ci, p, m + 1)], hi)], ci == 0))
                l0 += 128
                m += 1
    gas = [dict(wv=[1024 + 64 * h for h in range(4 * g, 4 * g + 4)], ktiles=ktiles, pairs=[
        dict(wq=128 * p, wk=(512 + 128 * p, 512 + 128 * p + 64), vslot=2 * (p % 2), gq=qn_a, gk=kn_a, sink=None,
             orow=128 * p, mi=2 * p, qtiles=qtiles) for p in (2 * g, 2 * g + 1)]) for g in range(2)]
    ktb = [(128 * j, 1, 128) for j in range(NE // 128 + 1)]
    qtb = []
    for m in range(NE // 128):
        kl = []
        if m > 0:
            kl.append((m - 1, 0))
        kl.append((m, 128))
        kl.append((m + 1, 256))
        qtb.append((128 * m, 1, 128, kl, True))
    gb = dict(wv=[2176, 2176, 2240, 2240], ktiles=ktb, pairs=[
        dict(wq=1536 + 128 * p, wk=(2048 + 64 * (p // 2), 2048 + 64 * (p // 2)), vslot=2 * (p // 2), gq=qn_b, gk=kn_b,
             sink=sinkp[p], orow=512 + 128 * p, mi=8 + 2 * p, qtiles=qtb) for p in range(4)])
    return gas + [gb]


def _masks_layer0():
    sa, sb_ = _alibi()
    kk = np.arange(128)[:, None].astype(np.float32)
    qq = np.arange(128)[None, :].astype(np.float32)
    out = np.full((16, 128, 768), NEGB, np.float32)
    for h in range(8):
        for ci, r in enumerate(DIL):
            d = 64 + qq - kk
            out[h, :, ci * 256:ci * 256 + 128] = np.where(np.abs(d) <= 64, -sa[h] * r * np.abs(d), NEGB)
            d = qq - kk - 64
            out[h, :, ci * 256 + 128:ci * 256 + 256] = np.where(np.abs(d) <= 64, -sa[h] * r * np.abs(d), NEGB)
    for h in range(8):
        for j, off in enumerate((128, 0, -128)):
            d = qq - kk + off
            out[8 + h, :, j * 128:(j + 1) * 128] = np.where(np.abs(d) <= 128, -sb_[h] * np.abs(d), NEGB)
    return out


def _slot1(o):
    return 0 if o == 3 else o + 2


def _heads_layer1(qn, kn):
    kt = [(128 * j, 1, 128) for j in range(NE // 128)]
    qt = []
    for m in range(NOWN // 128):
        ty = m if m < 2 else 2
        offs = range(0, 4) if m == 0 else range(-2, 3)
        kl = [(m + o, (ty * 5 + _slot1(o)) * 128) for o in offs if 0 <= m + o < NE // 128]
        qt.append((128 * m, 1, 128, kl, True))
    return [dict(wv=[2048 + 64 * h for h in range(16)], ktiles=kt, pairs=[
        dict(wq=128 * p, wk=(1024 + 128 * p, 1024 + 128 * p + 64), vslot=2 * p, gq=qn, gk=kn, sink=None,
             orow=128 * p, mi=2 * p, qtiles=qt) for p in range(8)])]


def _bias_layer1(rpb, hf):
    out = np.full((16, 128, 15 * 128), NEGB, np.float32)
    loc = np.arange(128)
    for ty in range(3):
        m = ty
        for o in (range(0, 4) if ty == 0 else range(-2, 3)):
            if m + o < 0:
                continue
            uq = 128 * m + loc
            uk = 128 * (m + o) + loc
            tq = uq if hf == 0 else 4095 - uq
            tk = uk if hf == 0 else 4095 - uk
            iq, cq = (tq // 64)[None, :], (tq % 64)[None, :]
            ik, ck = (tk // 64)[:, None], (tk % 64)[:, None]
            rs = np.clip(iq - 4, 0, 56)
            qs = np.clip(cq - 8, 0, 48)
            valid = (ik >= rs) & (ik < rs + 8) & (ck >= qs) & (ck < qs + 16)
            ro = np.clip(ik - iq + 7, 0, 14)
            co = np.clip(ck - cq, -15, 15) + 15
            g = rpb[:, ro, co]
            col = (ty * 5 + _slot1(o)) * 128
            out[:, :, col:col + 128] = np.where(valid[None], g, NEGB)
    return out


def _pk(v):
    return np.ascontiguousarray(np.asarray(v, np.float32).reshape(KC, 128).T)


def build_program(debug=False, upto=99):
    nc = bass.Bass("TRN2", target_bir_lowering=False)

    def din(name, shape, dt=F32):
        return nc.dram_tensor(name, shape, dt, kind="ExternalInput").ap()

    def dint(name, shape, dt):
        return nc.dram_tensor(name, shape, dt, kind="ExternalOutput" if debug else "Internal").ap()

    xT = din('xT', [D, NK])
    g1 = din('g1', [128, KC]); g2 = din('g2', [128, KC]); g3 = din('g3', [128, KC]); g4 = din('g4', [128, KC])
    w_in = din('w_in', [D, 2304]); w_o0 = din('w_o0', [D, D])
    qna = din('qna', [128, 1]); kna = din('kna', [128, 1]); qnb = din('qnb', [128, 1]); knb = din('knb', [128, 1])
    sink = din('sink', [4, 128, 1])
    fg = din('fg', [1, D, FF0]); fu = din('fu', [1, D, FF0]); fd = din('fd', [1, FF0, D])
    w_qkv = din('w_qkv', [D, 3072]); w_o1 = din('w_o1', [D, D])
    qnc = din('qnc', [128, 1]); knc = din('knc', [128, 1])
    m0 = din('m0', [16, 128, 768]); m1 = din('m1', [16, 128, 1920])
    rt = din('rt', [128, KC, NEXP])
    eg = din('eg', [NEXP, D, FF1]); eu = din('eu', [NEXP, D, FF1]); ed = din('ed', [NEXP, FF1, D])
    outT = nc.dram_tensor('outT', [D, NOWN], F32, kind="ExternalOutput").ap()
    h0 = dint('h0', [D, NK], BF16); x1 = dint('x1', [D, NE], F32); o0 = dint('o0', [D, NE], BF16)
    h1 = dint('h1', [D, NE], BF16); o1 = dint('o1', [D, NOWN], BF16)
    with ExitStack() as stack:
        S = Sched(nc, stack)
        if upto >= 1:
            norm_phase(nc, S, xT, h0, g1, NK)
        if upto >= 2:
            attn_phase(nc, S, h0, NK, NE, w_in, _heads_layer0(qna, kna, qnb, knb, sink), o0, m0, 768)
        if upto >= 4:
            ffn_phase(nc, S, xT, x1, g2, fg, fu, fd, FF0, [(0, 1152), (1152, 1152)], pmax=1152, tg=384,
                      oT=o0, wout=w_o0, hout=h1, gain2=g3)
        if upto >= 6:
            attn_phase(nc, S, h1, NE, NOWN, w_qkv, _heads_layer1(qnc, knc), o1, m1, 1920)
        if upto >= 8:
            ffn_phase(nc, S, x1, outT, g4, eg, eu, ed, FF1, [(0, 1024), (1024, 1024)], router=rt,
                      oT=o1, wout=w_o1)
    return nc


def kernel(x, ev_norm1, ev_w_in, ev_qn_a, ev_kn_a, ev_qn_b, ev_kn_b, ev_sink_b, ev_w_out,
           ev_norm2, ev_ffn_gate, ev_ffn_up, ev_ffn_down,
           od_norm1, od_w_qkv, od_qn, od_kn, od_rpb, od_w_out, od_norm2, od_router,
           od_exp_gate, od_exp_up, od_exp_down):
    f = lambda a: np.ascontiguousarray(np.asarray(a, np.float32))
    x = f(x)
    col = lambda v: f(np.tile(np.asarray(v).reshape(-1), 2).reshape(-1, 1))
    shared = dict(
        g1=_pk(ev_norm1[0]), g2=_pk(ev_norm2[0]), g3=_pk(od_norm1[0]), g4=_pk(od_norm2[0]),
        w_in=f(ev_w_in[0]), w_o0=f(ev_w_out[0]), qna=col(ev_qn_a[0]), kna=col(ev_kn_a[0]), qnb=col(ev_qn_b[0]),
        knb=col(ev_kn_b[0]), sink=f(np.repeat(np.asarray(ev_sink_b[0]).reshape(4, 2), 64, axis=1).reshape(4, 128, 1)), fg=f(ev_ffn_gate), fu=f(ev_ffn_up), fd=f(ev_ffn_down),
        w_qkv=f(od_w_qkv[0]), w_o1=f(od_w_out[0]), qnc=col(od_qn[0]), knc=col(od_kn[0]), m0=_masks_layer0(),
        rt=f(np.asarray(od_router[0]).reshape(KC, 128, NEXP).transpose(1, 0, 2)),
        eg=f(od_exp_gate[0]), eu=f(od_exp_up[0]), ed=f(od_exp_down[0]))
    rpb = np.asarray(od_rpb[0], np.float32)
    bias1 = [_bias_layer1(rpb, 0), _bias_layer1(rpb, 1)]
    in_maps = []
    for c in range(8):
        b, hf = c // 2, c % 2
        xs = x[b] if hf == 0 else x[b, ::-1]
        m = dict(shared)
        m['xT'] = np.ascontiguousarray(xs[:NK].T)
        m['m1'] = bias1[hf]
        in_maps.append(m)
    if _DEBUG_HOOK:
        return _DEBUG_HOOK[0](in_maps)
    nc = build_program()
    res = run_bass_kernel_spmd(nc, in_maps, core_ids=list(range(8)))
    out = np.empty((4, 4096, D), np.float32)
    for c in range(8):
        b, hf = c // 2, c % 2
        y = res.results[c]['outT'].T
        if hf == 0:
            out[b, :NOWN] = y
        else:
            out[b, NOWN:] = y[::-1]
    return out
```
